# Optimizing a Trainium2 kernel written in Bass

```python
import jax, jax.numpy as jnp
from jax import lax
import numpy as np

D_MODEL = 2048
BATCH = 2
SEQ = 4096
DEPTH = 1

MLA_HEADS = 8
MLA_Q_RANK = 512
MLA_KV_RANK = 256
MLA_NOPE = 128
MLA_ROPE = 64
MLA_V = 128
ROPE_THETA = 10000.0
Q_BLOCK = 128
GLA_HEADS = 4
GLA_DK = 128
GLA_DV = 256
GLA_GATE_RANK = 16
GLA_GATE_NORM = 16.0
GLA_CHUNK = 64
N_EXPERTS = 32
TOP_K = 4
D_EXPERT = D_MODEL
SWIGLU_LIMIT = 7.0
SWIGLU_ALPHA = 1.702
EXPERT_BLOCK = 256
EPS = 1e-6
N_MOD = 6

IN_SPLITS = (
    MLA_Q_RANK,
    MLA_KV_RANK + MLA_ROPE,
    GLA_HEADS * GLA_DK,
    GLA_HEADS * GLA_DK,
    GLA_HEADS * GLA_DV,
    GLA_GATE_RANK,
    GLA_HEADS * GLA_DV,
    D_MODEL,
    D_MODEL,
)
D_IN = sum(IN_SPLITS)
IN_OFFSETS = tuple(sum(IN_SPLITS[: i + 1]) for i in range(len(IN_SPLITS) - 1))

kernel_name = "hybrid_mla_gla_moe_adaln"


def rms_norm(x, w):
    xf = x.astype(jnp.float32)
    y = xf * lax.rsqrt(jnp.mean(xf * xf, axis=-1, keepdims=True) + EPS)
    return (y * w.astype(jnp.float32)).astype(x.dtype)


def modulate(h, shift, scale):
    return h * (1 + scale[:, None, :]) + shift[:, None, :]


def apply_rope(x, cos, sin):
    xf = x.astype(jnp.float32).reshape(*x.shape[:-1], MLA_ROPE // 2, 2)
    x_re, x_im = xf[..., 0], xf[..., 1]
    out = jnp.stack([x_re * cos - x_im * sin, x_re * sin + x_im * cos], axis=-1)
    return out.reshape(x.shape).astype(x.dtype)


def mla_attention(q_lat, kv_lat, positions, q_norm_w, w_q_b, kv_norm_w, w_kv_b):
    B, S, _ = q_lat.shape
    H = MLA_HEADS
    q = (rms_norm(q_lat, q_norm_w) @ w_q_b).reshape(B, S, H, MLA_NOPE + MLA_ROPE)
    q_nope, q_pe = q[..., :MLA_NOPE], q[..., MLA_NOPE:]
    c_kv, k_pe = kv_lat[..., :MLA_KV_RANK], kv_lat[..., MLA_KV_RANK:]
    kv = (rms_norm(c_kv, kv_norm_w) @ w_kv_b).reshape(B, S, H, MLA_NOPE + MLA_V)
    k_nope, v = kv[..., :MLA_NOPE], kv[..., MLA_NOPE:]
    inv_freq = ROPE_THETA ** (-(jnp.arange(0, MLA_ROPE, 2, dtype=jnp.float32) / MLA_ROPE))
    ang = positions.astype(jnp.float32)[..., None] * inv_freq
    cos, sin = jnp.cos(ang), jnp.sin(ang)
    q_pe = apply_rope(q_pe, cos[:, :, None, :], sin[:, :, None, :])
    k_pe = apply_rope(k_pe, cos, sin)
    scale = (MLA_NOPE + MLA_ROPE) ** -0.5
    nb = S // Q_BLOCK
    qn_blocks = q_nope.reshape(B, nb, Q_BLOCK, H, MLA_NOPE).transpose(1, 0, 2, 3, 4)
    qp_blocks = q_pe.reshape(B, nb, Q_BLOCK, H, MLA_ROPE).transpose(1, 0, 2, 3, 4)
    key_idx = jnp.arange(S)

    def query_block(args):
        qn, qp, blk = args
        s = jnp.einsum('bqhd,bkhd->bhqk', qn, k_nope, preferred_element_type=jnp.float32)
        s = s + jnp.einsum('bqhr,bkr->bhqk', qp, k_pe, preferred_element_type=jnp.float32)
        q_idx = blk * Q_BLOCK + jnp.arange(Q_BLOCK)
        mask = key_idx[None, :] <= q_idx[:, None]
        s = jnp.where(mask, s * scale, -jnp.inf)
        p = jax.nn.softmax(s, axis=-1).astype(v.dtype)
        return jnp.einsum('bhqk,bkhd->bqhd', p, v)

    o = lax.map(query_block, (qn_blocks, qp_blocks, jnp.arange(nb)))
    return o.transpose(1, 0, 2, 3, 4).reshape(B, S, H * MLA_V)


def gla_attention(q, k, v, gate_lr, g_out, w_gk_up, b_gk_up, norm_w):
    B, S, _ = q.shape
    H, DK, DV, C = GLA_HEADS, GLA_DK, GLA_DV, GLA_CHUNK
    n_chunks = S // C
    gk = jax.nn.log_sigmoid((gate_lr @ w_gk_up + b_gk_up).astype(jnp.float32)) / GLA_GATE_NORM

    def to_chunks(t, d):
        return t.astype(jnp.float32).reshape(B, n_chunks, C, H, d).transpose(1, 0, 3, 2, 4)

    qc = to_chunks(q, DK) * (DK ** -0.5)
    kc, vc, gc = to_chunks(k, DK), to_chunks(v, DV), to_chunks(gk, DK)
    causal = jnp.tril(jnp.ones((C, C), dtype=bool))

    def chunk_step(state, inp):
        qi, ki, vi, gi = inp
        b = jnp.cumsum(gi, axis=2)
        o_inter = jnp.einsum('bhcd,bhdv->bhcv', qi * jnp.exp(b), state)
        diff = b[:, :, :, None, :] - b[:, :, None, :, :]
        decay = jnp.exp(jnp.where(causal[:, :, None], diff, -jnp.inf))
        a = jnp.einsum('bhid,bhjd,bhijd->bhij', qi, ki, decay)
        o_intra = jnp.einsum('bhij,bhjv->bhiv', a, vi)
        b_last = b[:, :, -1:, :]
        state = state * jnp.exp(b_last[:, :, 0, :])[..., None] + jnp.einsum(
            'bhjd,bhjv->bhdv', ki * jnp.exp(b_last - b), vi)
        return state, o_inter + o_intra

    s0 = jnp.zeros((B, H, DK, DV), jnp.float32)
    _, o = lax.scan(chunk_step, s0, (qc, kc, vc, gc))
    o = o.transpose(1, 0, 3, 2, 4).reshape(B, S, H, DV)
    o = rms_norm(o, norm_w) * jax.nn.silu(g_out.astype(jnp.float32).reshape(B, S, H, DV))
    return o.reshape(B, S, H * DV).astype(q.dtype)


def moe_ffn(h, w_router, b_router, w1, b1, w2, b2):
    B, S, D = h.shape
    N = B * S
    E, BLK = N_EXPERTS, EXPERT_BLOCK
    xt = h.reshape(N, D)
    logits = (xt @ w_router + b_router).astype(jnp.float32)
    top_val, top_idx = lax.top_k(logits, TOP_K)
    top_w = jax.nn.softmax(top_val, axis=-1)
    A = N * TOP_K
    e_flat = top_idx.reshape(A)
    tok_flat = jnp.repeat(jnp.arange(N, dtype=jnp.int32), TOP_K)
    w_flat = top_w.reshape(A)
    onehot = jax.nn.one_hot(e_flat, E, dtype=jnp.int32)
    counts = onehot.sum(axis=0)
    rank = (jnp.cumsum(onehot, axis=0) * onehot).sum(axis=-1) - 1
    padded = (counts + BLK - 1) // BLK * BLK
    pad_end = jnp.cumsum(padded)
    pad_start = pad_end - padded
    dest = pad_start[e_flat] + rank
    n_blocks = -(-A // BLK) + E
    P = n_blocks * BLK
    slot_tok = jnp.full((P,), N, dtype=jnp.int32).at[dest].set(tok_flat)
    slot_w = jnp.zeros((P,), jnp.float32).at[dest].set(w_flat)
    block_expert = jnp.minimum(
        jnp.searchsorted(pad_end, jnp.arange(n_blocks) * BLK, side='right'), E - 1)
    x_pad = jnp.concatenate([xt, jnp.zeros((1, D), xt.dtype)], axis=0)
    xs = x_pad[slot_tok].reshape(n_blocks, BLK, D)

    def expert_block(args):
        xb, e = args
        hm = xb @ w1[e] + b1[e]
        x_glu, x_lin = hm[:, ::2], hm[:, 1::2]
        x_glu = jnp.minimum(x_glu, SWIGLU_LIMIT)
        x_lin = jnp.clip(x_lin, -SWIGLU_LIMIT, SWIGLU_LIMIT)
        act = x_glu * jax.nn.sigmoid(SWIGLU_ALPHA * x_glu) * (x_lin + 1)
        return act @ w2[e] + b2[e]

    ys = lax.map(expert_block, (xs, block_expert)).reshape(P, D)
    out = jnp.zeros((N + 1, D), jnp.float32).at[slot_tok].add(
        ys.astype(jnp.float32) * slot_w[:, None])
    return out[:N].reshape(B, S, D).astype(h.dtype)


def setup_inputs(seed: int = 0) -> dict:
    key = jax.random.key(seed)
    ks = jax.random.split(key, 32)
    f32 = jnp.float32
    L, D = DEPTH, D_MODEL

    def nrm(k, shape, fan_in):
        return jax.random.normal(k, shape, f32) * (fan_in ** -0.5)

    def gain(k, shape):
        return 1.0 + 0.02 * jax.random.normal(k, shape, f32)

    def bias(k, shape):
        return 0.01 * jax.random.normal(k, shape, f32)

    x = jax.random.normal(ks[0], (BATCH, SEQ, D), f32)
    c = jax.random.normal(ks[1], (BATCH, D), f32)
    offsets = jax.random.randint(ks[2], (BATCH, 1), 0, 1024, dtype=jnp.int32)
    positions = offsets + jnp.arange(SEQ, dtype=jnp.int32)[None, :]
    return {
        "x": x,
        "c": c,
        "positions": positions,
        "w_ada": 0.5 * nrm(ks[3], (L, D, N_MOD * D), D),
        "b_ada": bias(ks[4], (L, N_MOD * D)),
        "norm_mix_w": gain(ks[5], (L, D)),
        "w_in": nrm(ks[6], (L, D, D_IN), D),
        "mla_q_norm_w": gain(ks[7], (L, MLA_Q_RANK)),
        "mla_w_q_b": nrm(ks[8], (L, MLA_Q_RANK, MLA_HEADS * (MLA_NOPE + MLA_ROPE)), MLA_Q_RANK),
        "mla_kv_norm_w": gain(ks[9], (L, MLA_KV_RANK)),
        "mla_w_kv_b": nrm(ks[10], (L, MLA_KV_RANK, MLA_HEADS * (MLA_NOPE + MLA_V)), MLA_KV_RANK),
        "gla_w_gk_up": nrm(ks[11], (L, GLA_GATE_RANK, GLA_HEADS * GLA_DK), GLA_GATE_RANK),
        "gla_b_gk_up": bias(ks[12], (L, GLA_HEADS * GLA_DK)),
        "gla_norm_w": gain(ks[13], (L, GLA_DV)),
        "w_o_mla": nrm(ks[14], (L, MLA_HEADS * MLA_V, D), MLA_HEADS * MLA_V),
        "w_o_gla": nrm(ks[15], (L, GLA_HEADS * GLA_DV, D), GLA_HEADS * GLA_DV),
        "w_out": nrm(ks[16], (L, D, D), D),
        "norm_ffn_w": gain(ks[17], (L, D)),
        "w_router": nrm(ks[18], (L, D, N_EXPERTS), D),
        "b_router": bias(ks[19], (L, N_EXPERTS)),
        "w1": nrm(ks[20], (L, N_EXPERTS, D, 2 * D_EXPERT), D),
        "b1": bias(ks[21], (L, N_EXPERTS, 2 * D_EXPERT)),
        "w2": nrm(ks[22], (L, N_EXPERTS, D_EXPERT, D), D_EXPERT),
        "b2": bias(ks[23], (L, N_EXPERTS, D)),
        "w_ada_final": 0.5 * nrm(ks[24], (D, 2 * D), D),
        "b_ada_final": bias(ks[25], (2 * D,)),
        "norm_final_w": gain(ks[26], (D,)),
    }


def reference(x, c, positions, w_ada, b_ada, norm_mix_w, w_in, mla_q_norm_w, mla_w_q_b,
              mla_kv_norm_w, mla_w_kv_b, gla_w_gk_up, gla_b_gk_up, gla_norm_w, w_o_mla,
              w_o_gla, w_out, norm_ffn_w, w_router, b_router, w1, b1, w2, b2,
              w_ada_final, b_ada_final, norm_final_w):
    cond = jax.nn.silu(c)
    for l in range(DEPTH):
        mod = cond @ w_ada[l] + b_ada[l]
        sh_m, sc_m, g_m, sh_f, sc_f, g_f = jnp.split(mod, N_MOD, axis=-1)
        h = modulate(rms_norm(x, norm_mix_w[l]), sh_m, sc_m)
        proj = h @ w_in[l]
        q_lat, kv_lat, gq, gk, gv, g_lr, g_out, gate_a, gate_b = jnp.split(proj, IN_OFFSETS, axis=-1)
        y_a = mla_attention(q_lat, kv_lat, positions, mla_q_norm_w[l], mla_w_q_b[l],
                            mla_kv_norm_w[l], mla_w_kv_b[l]) @ w_o_mla[l]
        y_b = gla_attention(gq, gk, gv, g_lr, g_out, gla_w_gk_up[l], gla_b_gk_up[l],
                            gla_norm_w[l]) @ w_o_gla[l]
        merged = jax.nn.sigmoid(gate_a) * y_a + jax.nn.sigmoid(gate_b) * y_b
        x = x + g_m[:, None, :] * (merged @ w_out[l])
        h = modulate(rms_norm(x, norm_ffn_w[l]), sh_f, sc_f)
        x = x + g_f[:, None, :] * moe_ffn(h, w_router[l], b_router[l], w1[l], b1[l], w2[l], b2[l])
    fmod = cond @ w_ada_final + b_ada_final
    sh_o, sc_o = jnp.split(fmod, 2, axis=-1)
    return modulate(rms_norm(x, norm_final_w), sh_o, sc_o)
```

```python
import os
from contextlib import ExitStack
import numpy as np
import concourse.bass as bass
import concourse.mybir as mybir
from concourse.bass_utils import run_bass_kernel_spmd

F32 = mybir.dt.float32
BF16 = mybir.dt.bfloat16
I32 = mybir.dt.int32
AF = mybir.ActivationFunctionType
ALU = mybir.AluOpType
AX = mybir.AxisListType

D = 2048
KD = 16
S = 4096
NT = 32
OWN0 = 24
NE = 32
EPS = 1e-6
C_QLAT, C_CKV, C_KPE, C_GQ, C_GK, C_GV, C_GLR, C_GOUT, C_GA, C_GB = 0, 512, 768, 832, 1344, 1856, 2880, 2896, 3920, 5968
WA_CKV, WA_KPE, WA_GK, WA_GV, WA_GLR, WA_GQ, WA_N = 0, 256, 320, 832, 1856, 1872, 2384

DEBUG = os.environ.get("MK_DEBUG", "")
N_EXPERTS_RUN = int(os.environ.get("MK_NEXP", "32"))
STOP = int(os.environ.get("MK_STOP", "99"))


class _S:
    def __init__(self, sem, selfsync=False, handle=None, name=""):
        self.sem = sem
        self.val = 0
        self.selfsync = selfsync
        self.h = handle
        self.seen = {}
        self.name = name


class _Reg:
    __slots__ = ("writers", "readers")

    def __init__(self):
        self.writers = {}
        self.readers = {}


class Prog:
    def __init__(self, nc, es):
        self.nc = nc
        self.es = es
        self.regs = {}
        self.dsems = {}
        self.groups = {}
        mk = lambda n: es.enter_context(nc.semaphore(n))
        self.pe = _S(mk("s_pe"), False, nc.tensor, "pe")
        self.act = _S(mk("s_act"), True, nc.scalar, "act")
        self.dve = _S(mk("s_dve"), True, nc.vector, "dve")
        self.pool = _S(mk("s_pool"), True, nc.gpsimd, "pool")
        self.sp = _S(mk("s_sp"), False, nc.sync, "sp")
        self.engs = [self.pe, self.act, self.dve, self.pool, self.sp]

    def _reg(self, k):
        r = self.regs.get(k)
        if r is None:
            r = self.regs[k] = _Reg()
        return r

    def _collect(self, E, r, w):
        need = {}
        for k in r:
            rg = self._reg(k)
            for S_, v in rg.writers.items():
                if need.get(S_, 0) < v:
                    need[S_] = v
            if isinstance(k, tuple) and k[0] == "ps":
                for S_, v in rg.readers.items():
                    if S_ is not E and need.get(S_, 0) < v:
                        need[S_] = v
        for k in w:
            rg = self._reg(k)
            for S_, v in rg.writers.items():
                if need.get(S_, 0) < v:
                    need[S_] = v
            for S_, v in rg.readers.items():
                if need.get(S_, 0) < v:
                    need[S_] = v
        for S_, v in need.items():
            if S_ is E and not E.selfsync:
                continue
            if E.seen.get(S_, 0) >= v:
                continue
            E.seen[S_] = v
            E.h.wait_ge(S_.sem, v)

    def _update(self, S_, val, r, w):
        for k in r:
            self._reg(k).readers[S_] = val
        for k in w:
            rg = self._reg(k)
            rg.writers = {S_: val}
            rg.readers = {}

    def op(self, E, fn, r=(), w=()):
        self._collect(E, r, w)
        ins = fn(E.h)
        E.val += 1
        ins.then_inc(E.sem, 1)
        self._update(E, E.val, r, w)

    def PE(self, fn, r=(), w=()):
        self.op(self.pe, fn, r, w)

    def ACT(self, fn, r=(), w=()):
        self.op(self.act, fn, r, w)

    def DVE(self, fn, r=(), w=()):
        self.op(self.dve, fn, r, w)

    def POOL(self, fn, r=(), w=()):
        self.op(self.pool, fn, r, w)

    def dma(self, q, out, in_, r=(), w=(), semkey=None, **kw):
        E = self.pool if q == "pool" else self.sp
        if semkey is None:
            semkey = ("d", w[0] if w else r[0])
        ds = self.dsems.get(semkey)
        if ds is None:
            ds = self.dsems[semkey] = _S(self.es.enter_context(self.nc.semaphore("sd%d" % len(self.dsems))), name=str(semkey))
        self._collect(E, r, w)
        E.h.dma_start(out=out, in_=in_, **kw).then_inc(ds.sem, 16)
        ds.val += 16
        self._update(ds, ds.val, r, w)
        self.groups.setdefault(semkey, []).extend(list(w))
        return ds

    def finalize(self, semkey):
        ds = self.dsems[semkey]
        for k in self.groups.get(semkey, []):
            rg = self._reg(k)
            if ds in rg.writers:
                rg.writers[ds] = ds.val

    def barrier(self):
        allS = self.engs[:4] + list(self.dsems.values())
        for E in self.engs:
            for S_ in allS:
                if S_ is E or S_.val == 0:
                    continue
                if E.seen.get(S_, 0) >= S_.val:
                    continue
                E.seen[S_] = S_.val
                E.h.wait_ge(S_.sem, S_.val)

    def final_wait(self):
        E = self.sp
        for S_ in self.engs[:4] + list(self.dsems.values()):
            if S_.val and E.seen.get(S_, 0) < S_.val:
                E.h.wait_ge(S_.sem, S_.val)


def build_nc():
    nc = bass.Bass("TRN2", target_bir_lowering=False)
    dt = lambda n, s, d=F32, k="ExternalInput": nc.dram_tensor(n, list(s), d, kind=k).ap()
    xs = dt("xs", [S, D])
    posr = dt("posr", [64, S], I32)
    valid = dt("valid", [128, NT])
    cT = dt("cT", [128, KD])
    w_ada = dt("w_ada", [D, 6 * D])
    b_ada = dt("b_ada", [96, 128])
    w_ada_f = dt("w_ada_final", [D, 2 * D])
    b_ada_f = dt("b_ada_final", [32, 128])
    vecs = dt("vecs", [54, 128])
    w_in = dt("w_in", [D, 8016])
    w_q_b = dt("mla_w_q_b", [512, 1536])
    w_kv_b = dt("mla_w_kv_b", [256, 2048])
    wgk1 = dt("wgk1", [17, 512])
    gla_nw = dt("gla_nw", [128, 256])
    w_o_mla = dt("w_o_mla", [1024, D])
    w_o_gla = dt("w_o_gla", [1024, D])
    w_out = dt("w_out", [D, D])
    w_router = dt("w_router", [D, NE])
    b_router = dt("b_router", [128, NE])
    if STOP > 4:
        w1 = dt("w1", [NE, D, 2 * D])
        b1 = dt("b1", [NE, 2 * D])
        w2 = dt("w2", [NE, D, D])
        b2 = dt("b2", [NE, D])
    c_ident = dt("c_ident", [128, 128])
    c_tri = dt("c_tri", [128, 256])
    c_mask = dt("c_mask", [128, 4 * 512])
    c_rope = dt("c_rope", [64, 2])
    out = dt("out", [1024, D], F32, "ExternalOutput")
    x1d = dt("x1d", [1024, D], F32, "Internal")
    glao_d = dt("glao_d", [128, 8192], BF16, "Internal")
    mlao_d = dt("mlao_d", [128, 8192], BF16, "Internal")
    dbg = {}
    if DEBUG:
        for nm, shp in [("d_mod", [128, 128]), ("d_hT", [128, 16 * 128]), ("d_ckvn", [128, 2 * S]), ("d_krot", [64, S]),
                        ("d_S", [128, 1024]), ("d_on", [128, 8 * 1024]), ("d_glao", [128, 8 * 1024]),
                        ("d_qn", [128, 8 * 1024]), ("d_qrot", [64, 8 * 1024]), ("d_mlao", [128, 8 * 1024]),
                        ("d_merged", [128, 16 * 1024]), ("d_x1", [128, 8 * 2048]), ("d_G", [128, 8 * 32]),
                        ("d_acc", [128, 8 * 2048])]:
            dbg[nm] = dt(nm, shp, F32, "ExternalOutput")

    with ExitStack() as es:
        P = Prog(nc, es)
        sb = lambda n, s, d=F32, st=es: st.enter_context(nc.sbuf_tensor(n, list(s), d))
        pst = lambda n, s, d=F32, st=es: st.enter_context(nc.psum_tensor(n, list(s), d))
        block = es.enter_context(nc.Block())

        def body(_):
            ident = sb("ident", [128, 128])
            identb = sb("identb", [128, 128], BF16)
            ones_f = sb("ones_f", [128, 128])
            ones_b = sb("ones_b", [128, 128], BF16)
            tri = sb("tri", [128, 256])
            cmask = sb("cmask", [128, 2048], BF16)
            crope = sb("crope", [64, 2])
            valid_sb = sb("valid_sb", [128, NT])
            kbias = sb("kbias", [128, NT])
            modT = sb("modT", [128, 128])
            vT = sb("vT", [128, 64])
            AB = sb("AB", [128, 6 * 16])
            wgk1_sb = sb("wgk1_sb", [17, 512])
            glanw = sb("glanw", [128, 256])
            brout = sb("brout", [128, NE])
            G = sb("G", [128, 8, NE])
            eps_t = sb("eps_t", [128, 1])
            one_t = sb("one_t", [128, 1])
            PS = [pst("ps%d" % i, [128, 512]) for i in range(8)]
            pk = lambda i: ("ps", i)

            def dump(name, ap, key, cols):
                if not DEBUG or name not in dbg:
                    return
                for c0 in range(0, cols, 2048):
                    c1 = min(cols, c0 + 2048)
                    P.dma("pool", dbg[name][0:ap.shape[0], c0:c1], ap[:, c0:c1], r=[key], semkey=("dbg", name))

            ck = "const"
            P.dma("sp", ident[:], c_ident[:, :], w=["ident"], semkey=ck)
            P.dma("sp", tri[:], c_tri[:, :], w=["tri"], semkey=ck)
            P.dma("sp", crope[:], c_rope[:, :], w=["crope"], semkey=ck)
            P.dma("sp", valid_sb[:], valid[:, :], w=["valid"], semkey=ck)
            P.dma("sp", wgk1_sb[:], wgk1[:, :], w=["wgk1"], semkey=ck)
            P.dma("sp", glanw[:], gla_nw[:, :], w=["glanw"], semkey=ck)
            P.dma("sp", brout[:], b_router[:, :], w=["brout"], semkey=ck)
            P.dma("pool", cmask[:], c_mask[:, :], w=["cmask"], semkey="constp")
            P.finalize(ck)
            P.finalize("constp")
            P.DVE(lambda e: e.memset(ones_f[:], 1.0), w=["ones_f"])
            P.DVE(lambda e: e.memset(ones_b[:], 1.0), w=["ones_b"])
            P.DVE(lambda e: e.memset(eps_t[:], EPS), w=["eps_t"])
            P.DVE(lambda e: e.memset(one_t[:], 1.0), w=["one_t"])
            P.DVE(lambda e: e.tensor_copy(out=identb[:], in_=ident[:]), r=["ident"], w=["identb"])
            P.DVE(lambda e: e.tensor_scalar(out=kbias[:], in0=valid_sb[:], scalar1=-1.0, scalar2=30000.0,
                                            op0=ALU.add, op1=ALU.mult), r=["valid"], w=["kbias"])

            with ExitStack() as ph:
                cin = sb("cin", [128, KD], F32, ph)
                cond = sb("cond", [128, KD], BF16, ph)
                vrow = sb("vrow", [128, 128], F32, ph)
                vrow2 = sb("vrow2", [54, 128], F32, ph)
                wbuf = [sb("wada%d" % i, [128, KD, 512], BF16, ph) for i in range(2)]
                P.dma("sp", cin[:], cT[:, :], w=["cin"])
                P.dma("sp", vrow[0:96, :], b_ada[:, :], w=["vrow"], semkey="vr")
                P.dma("sp", vrow[96:128, :], b_ada_f[:, :], w=["vrow"], semkey="vr")
                P.dma("sp", vrow2[:], vecs[:, :], w=["vrow2"], semkey="vr")
                P.finalize("vr")
                P.ACT(lambda e: e.activation(out=cond[:], in_=cin[:], func=AF.Silu), r=["cin"], w=["cond"])
                nblk = 32
                for bi in range(nblk):
                    wb = wbuf[bi % 2]
                    src = (w_ada[:, bi * 512:(bi + 1) * 512] if bi < 24 else w_ada_f[:, (bi - 24) * 512:(bi - 23) * 512])
                    P.dma("pool", wb[:], src.rearrange("(k p) n -> p k n", p=128), w=[("wada", bi % 2)])
                    for jj in range(4):
                        j = bi * 4 + jj
                        for k in range(KD):
                            P.PE(lambda e, k=k, jj=jj, j=j, wb=wb: e.matmul(
                                PS[0][:, j:j + 1], wb[:, k, jj * 128:(jj + 1) * 128], cond[:, k:k + 1],
                                start=(k == 0), stop=(k == KD - 1)),
                                r=[("wada", bi % 2), "cond"], w=[pk(0)])
                P.PE(lambda e: e.transpose(PS[1][:, 0:128], vrow[:], ident[:]), r=["vrow", "ident"], w=[pk(1)])
                P.PE(lambda e: e.transpose(PS[1][:, 128:128 + 54], vrow2[:], ident[0:54, 0:54]), r=["vrow2", "ident"], w=[pk(1)])
                P.ACT(lambda e: e.activation(out=vT[:, 0:54], in_=PS[1][:, 128:128 + 54], func=AF.Copy), r=[pk(1)], w=["vT"])
                P.ACT(lambda e: e.activation(func=AF.Copy, out=modT[:], in_=PS[1][:, 0:128]), r=[pk(1)], w=["modT"])
                P.DVE(lambda e: e.tensor_tensor(out=modT[:], in0=PS[0][:, 0:128], in1=modT[:], op=ALU.add),
                      r=[pk(0), "modT"], w=["modT"])
                for idx, (nw0, sc0, sh0) in enumerate([(0, 16, 0), (16, 64, 48), (32, 112, 96)]):
                    a_ap = AB[:, (2 * idx) * 16:(2 * idx + 1) * 16]
                    b_ap = AB[:, (2 * idx + 1) * 16:(2 * idx + 2) * 16]
                    P.DVE(lambda e, a_ap=a_ap, sc0=sc0, nw0=nw0: e.scalar_tensor_tensor(
                        out=a_ap, in0=modT[:, sc0:sc0 + 16], scalar=1.0, in1=vT[:, nw0:nw0 + 16],
                        op0=ALU.add, op1=ALU.mult), r=["modT", "vT"], w=["AB"])
                    P.DVE(lambda e, b_ap=b_ap, sh0=sh0: e.tensor_copy(out=b_ap, in_=modT[:, sh0:sh0 + 16]),
                          r=["modT"], w=["AB"])
                dump("d_mod", modT[:], "modT", 128)
                P.barrier()

            if STOP <= 0:
                return
            A_m, B_m = AB[:, 0:16], AB[:, 16:32]
            A_f, B_f = AB[:, 32:48], AB[:, 48:64]
            A_o, B_o = AB[:, 64:80], AB[:, 80:96]
            qnw = vT[:, 48:52]
            kvnw = vT[:, 52:54]

            def make_hT(xt, xk, dst_fn, dkeys, Acol, Bcol, stat, f32dst_fn=None, f32keys=None):
                junk = stat["junk"]
                P.ACT(lambda e: e.activation(out=junk[:], in_=xt, func=AF.Square, accum_out=stat["t"][:, 0:1]),
                      r=[xk], w=["junk", "stat"])
                P.ACT(lambda e: e.activation(out=stat["t"][:, 1:2], in_=stat["t"][:, 0:1], func=AF.Sqrt,
                                             bias=eps_t[:], scale=1.0 / D), r=["stat", "eps_t"], w=["stat"])
                P.DVE(lambda e: e.reciprocal(out=stat["t"][:, 2:3], in_=stat["t"][:, 1:2]), r=["stat"], w=["stat"])
                P.DVE(lambda e: e.tensor_scalar(out=xt, in0=xt, scalar1=stat["t"][:, 2:3], scalar2=None, op0=ALU.mult),
                      r=[xk, "stat"], w=[xk])
                for g4 in range(4):
                    bank = 4 + (g4 % 2)
                    for i in range(4):
                        c = g4 * 4 + i
                        P.PE(lambda e, c=c, i=i, bank=bank: e.transpose(PS[bank][:, i * 128:(i + 1) * 128],
                                                                          xt[:, c * 128:(c + 1) * 128], ident[:]),
                             r=[xk, "ident"], w=[pk(bank)])
                    for i in range(4):
                        c = g4 * 4 + i
                        src = PS[bank][:, i * 128:(i + 1) * 128]
                        if i % 2 == 0:
                            P.ACT(lambda e, c=c, src=src: e.activation(out=dst_fn(c), in_=src, func=AF.Identity,
                                                                       bias=Bcol[:, c:c + 1], scale=Acol[:, c:c + 1]),
                                  r=[pk(bank), "AB"], w=[dkeys[c]])
                        else:
                            P.DVE(lambda e, c=c, src=src: e.tensor_scalar(out=dst_fn(c), in0=src, scalar1=Acol[:, c:c + 1],
                                                                          scalar2=Bcol[:, c:c + 1], op0=ALU.mult, op1=ALU.add),
                                  r=[pk(bank), "AB"], w=[dkeys[c]])
                        if f32dst_fn is not None:
                            P.DVE(lambda e, c=c, src=src: e.tensor_scalar(out=f32dst_fn(c), in0=src, scalar1=Acol[:, c:c + 1],
                                                                          scalar2=Bcol[:, c:c + 1], op0=ALU.mult, op1=ALU.add),
                                  r=[pk(bank), "AB"], w=[f32keys[c]])

            mix = ExitStack()
            ckvn = sb("ckvn", [128, 2, S], BF16, mix)
            krot = sb("krot", [128, S], BF16, mix)
            o_n = sb("o_n", [128, 8, 1024], BF16, mix)
            cs_own = sb("cs_own", [64, 2, 1024], F32, mix)

            with ExitStack() as ph:
                WA = sb("WA", [128, KD, WA_N], BF16, ph)
                xt_b = [sb("xt%d" % i, [128, D], F32, ph) for i in range(2)]
                hT_b = [sb("hT%d" % i, [128, KD, 128], BF16, ph) for i in range(2)]
                junk = sb("junk", [128, D], BF16, ph)
                statt = sb("statt", [128, 4], F32, ph)
                stat = {"junk": junk, "t": statt}
                posi = sb("posi", [64, 128], I32, ph)
                ang = sb("ang", [64, 6, 128], F32, ph)
                ckvf = sb("ckvf", [128, 2, 128], F32, ph)
                sq = sb("sq", [128, 2, 128], BF16, ph)
                rinv = sb("rinv", [128, 2, 128], F32, ph)
                rtmp = sb("rtmp", [64, 2, 128], F32, ph)
                glrT1 = sb("glrT1", [17, 128], F32, ph)
                lsp = sb("lsp", [128, 2, 512], F32, ph)
                ek = sb("ek", [128, 512], F32, ph)
                kp_tok = sb("kp_tok", [128, 512], BF16, ph)
                v_tok = sb("v_tok", [128, 1024], BF16, ph)
                ebT = sb("ebT", [128, 2, 512], F32, ph)
                qpT = sb("qpT", [128, 512], BF16, ph)
                kppT = sb("kppT", [128, 512], BF16, ph)
                AT = sb("AT", [128, 512], BF16, ph)
                Sst = sb("Sst", [128, 1024], F32, ph)
                Sbf = sb("Sbf", [128, 1024], BF16, ph)
                ostat = sb("ostat", [128, 16], F32, ph)
                otmp = sb("otmp", [128, 1024], F32, ph)
                for (a, b_, c0) in [(WA_CKV, 320, C_CKV), (WA_GK, 512, C_GK), (WA_GV, 512, C_GV), (WA_GV + 512, 512, C_GV + 512),
                                    (WA_GLR, 16, C_GLR), (WA_GQ, 512, C_GQ)]:
                    P.dma("pool", WA[:, :, a:a + b_], w_in[:, c0:c0 + b_].rearrange("(k p) n -> p k n", p=128),
                          w=["WA"], semkey="WA")
                P.finalize("WA")
                P.DVE(lambda e: e.memset(glrT1[:], 1.0), w=["glrT1"])
                P.DVE(lambda e: e.memset(krot[64:128, :], 0.0), w=["krotpad"])
                P.DVE(lambda e: e.memset(Sst[:], 0.0), w=["Sst"])
                P.DVE(lambda e: e.memset(Sbf[:], 0.0), w=["Sbf"])
                hkeys = lambda bi: [("hT", bi, c) for c in range(KD)]
                def prep(t):
                    bi = t % 2
                    xt, hT = xt_b[bi], hT_b[bi]
                    xk = ("xt", bi)
                    P.dma("sp", xt[:], xs[t * 128:(t + 1) * 128, :], w=[xk])
                    make_hT(xt[:], xk, lambda c, hT=hT: hT[:, c, :], hkeys(bi), A_m, B_m, stat)

                prep(0)
                for t in range(NT):
                    own = t >= OWN0
                    bi = t % 2
                    xt, hT = xt_b[bi], hT_b[bi]
                    xk = ("xt", bi)
                    if t + 1 < NT:
                        prep(t + 1)
                    HK = hkeys(bi)
                    P.dma("sp", posi[:], posr[:, t * 128:(t + 1) * 128], w=["posi"])
                    a0, a1, a2, a3, a4, a5 = [ang[:, i, :] for i in range(6)]
                    P.DVE(lambda e: e.tensor_copy(out=a0, in_=posi[:]), r=["posi"], w=["ang0"])
                    P.DVE(lambda e: e.tensor_scalar(out=a0, in0=a0, scalar1=crope[:, 0:1], scalar2=None, op0=ALU.mult),
                          r=["ang0", "crope"], w=["ang0"])
                    MAGIC = 12582912.0
                    C1, C2 = 6.28125, 0.0019353071795864769
                    for which, off, dstc in [("sin", 0.0, a4), ("cos", 0.25, a5)]:
                        P.DVE(lambda e, off=off: e.tensor_scalar(out=a1, in0=a0, scalar1=1.0 / (2 * np.pi), scalar2=off,
                                                                 op0=ALU.mult, op1=ALU.add), r=["ang0"], w=["ang1"])
                        P.DVE(lambda e: e.tensor_scalar(out=a1, in0=a1, scalar1=MAGIC, scalar2=None, op0=ALU.add),
                              r=["ang1"], w=["ang1"])
                        P.DVE(lambda e: e.tensor_scalar(out=a1, in0=a1, scalar1=-MAGIC, scalar2=None, op0=ALU.add),
                              r=["ang1"], w=["ang1"])
                        P.DVE(lambda e: e.scalar_tensor_tensor(out=a2, in0=a1, scalar=-C1, in1=a0, op0=ALU.mult, op1=ALU.add),
                              r=["ang1", "ang0"], w=["ang2"])
                        P.DVE(lambda e: e.scalar_tensor_tensor(out=a2, in0=a1, scalar=-C2, in1=a2, op0=ALU.mult, op1=ALU.add),
                              r=["ang1", "ang2"], w=["ang2"])
                        P.DVE(lambda e, off=off: e.tensor_scalar(out=a2, in0=a2, scalar1=off * 2 * np.pi, scalar2=3.1415925,
                                                                 op0=ALU.add, op1=ALU.min), r=["ang2"], w=["ang2"])
                        P.DVE(lambda e: e.tensor_scalar(out=a2, in0=a2, scalar1=-3.1415925, scalar2=None, op0=ALU.max),
                              r=["ang2"], w=["ang2"])
                        P.ACT(lambda e, dstc=dstc: e.activation(out=dstc, in_=a2, func=AF.Sin), r=["ang2"], w=["angcs"])
                    P.DVE(lambda e: e.tensor_scalar(out=a4, in0=a4, scalar1=crope[:, 1:2], scalar2=None, op0=ALU.mult),
                          r=["angcs", "crope"], w=["angcs"])
                    if own:
                        oo = (t - OWN0) * 128
                        P.DVE(lambda e, oo=oo: e.tensor_copy(out=cs_own[:, 0, oo:oo + 128], in_=a5), r=["angcs"], w=["cs_own"])
                        P.DVE(lambda e, oo=oo: e.tensor_copy(out=cs_own[:, 1, oo:oo + 128], in_=a4), r=["angcs"], w=["cs_own"])
                    for c in range(2):
                        for k in range(KD):
                            P.PE(lambda e, c=c, k=k: e.matmul(PS[0][:, c * 128:(c + 1) * 128], WA[:, k, WA_CKV + c * 128:WA_CKV + (c + 1) * 128],
                                                               hT[:, k, :], start=(k == 0), stop=(k == KD - 1)),
                                 r=["WA"] + HK, w=[pk(0)])
                    for v_ in range(2):
                        for half in range(2):
                            par = half if v_ == 0 else 1 - half
                            for k in range(KD):
                                lhs = WA[:, k, WA_KPE + par:WA_KPE + 64:2]
                                P.PE(lambda e, v_=v_, k=k, lhs=lhs, half=half: e.matmul(
                                    PS[1][half * 32:(half + 1) * 32, v_ * 128:(v_ + 1) * 128], lhs, hT[:, k, :],
                                    start=(k == 0), stop=(k == KD - 1)), r=["WA"] + HK, w=[pk(1)])
                    P.ACT(lambda e: e.activation(func=AF.Copy, out=ckvf[:].rearrange("p c t -> p (c t)"), in_=PS[0][:, 0:256]), r=[pk(0)], w=["ckvf"])
                    P.ACT(lambda e: e.activation(out=sq[:].rearrange("p c t -> p (c t)"), in_=PS[0][:, 0:256], func=AF.Square),
                          r=[pk(0)], w=["sq"])
                    for c in range(2):
                        P.PE(lambda e, c=c: e.matmul(PS[2][:, 0:128], ones_b[:], sq[:, c, :], start=(c == 0), stop=(c == 1)),
                             r=["ones_b", "sq"], w=[pk(2)])
                    P.ACT(lambda e: e.activation(out=rinv[:, 0, :], in_=PS[2][:, 0:128], func=AF.Sqrt, bias=eps_t[:], scale=1.0 / 256),
                          r=[pk(2), "eps_t"], w=["rinv"])
                    P.DVE(lambda e: e.reciprocal(out=rinv[:, 1, :], in_=rinv[:, 0, :]), r=["rinv"], w=["rinv"])
                    for c in range(2):
                        P.DVE(lambda e, c=c: e.scalar_tensor_tensor(out=ckvn[:, c, t * 128:(t + 1) * 128], in0=ckvf[:, c, :],
                                                                   scalar=kvnw[:, c:c + 1], in1=rinv[:, 1, :], op0=ALU.mult, op1=ALU.mult),
                              r=["ckvf", "rinv", "vT"], w=[("ckvn", t)])
                    P.DVE(lambda e: e.tensor_tensor(out=rtmp[:, 0, :], in0=PS[1][0:64, 0:128], in1=a5, op=ALU.mult),
                          r=[pk(1), "angcs"], w=["rtmp"])
                    P.DVE(lambda e: e.tensor_tensor(out=rtmp[:, 1, :], in0=PS[1][0:64, 128:256], in1=a4, op=ALU.mult),
                          r=[pk(1), "angcs"], w=["rtmp"])
                    P.DVE(lambda e: e.tensor_tensor(out=krot[0:64, t * 128:(t + 1) * 128], in0=rtmp[:, 0, :], in1=rtmp[:, 1, :], op=ALU.add),
                          r=["rtmp"], w=[("krot", t)])
                    for k in range(KD):
                        P.PE(lambda e, k=k: e.matmul(PS[2][0:16, 128:256], WA[:, k, WA_GLR:WA_GLR + 16], hT[:, k, :],
                                                     start=(k == 0), stop=(k == KD - 1)), r=["WA"] + HK, w=[pk(2)])
                    P.ACT(lambda e: e.activation(out=glrT1[0:16, :], in_=PS[2][0:16, 128:256], func=AF.Copy), r=[pk(2)], w=["glrT1"])
                    P.PE(lambda e: e.matmul(PS[3][:, :], glrT1[:], wgk1_sb[:], start=True, stop=True), r=["glrT1", "wgk1"], w=[pk(3)])
                    P.ACT(lambda e: e.activation(out=lsp[:, 0, :], in_=PS[3][:, :], func=AF.Exp, scale=-1.0), r=[pk(3)], w=["lsp0"])
                    P.ACT(lambda e: e.activation(out=lsp[:, 1, :], in_=lsp[:, 0, :], func=AF.Ln, bias=one_t[:], scale=1.0),
                          r=["lsp0", "one_t"], w=["lsp"])
                    L = lsp[:, 1, :]
                    P.PE(lambda e: e.matmul(PS[3][:, :], tri[:, 128:256], L, start=True, stop=True), r=["tri", "lsp"], w=[pk(3)])
                    for hd in range(4):
                        P.PE(lambda e, hd=hd: e.matmul(PS[2][:, hd * 128:(hd + 1) * 128], L[:, hd * 128:(hd + 1) * 128], tri[:, 0:128],
                                                       start=True, stop=True), r=["tri", "lsp"], w=[pk(2)])
                    P.ACT(lambda e: e.activation(out=ek[:], in_=PS[3][:, :], func=AF.Exp), r=[pk(3)], w=["ek"])
                    P.ACT(lambda e: e.activation(out=ebT[:, 0, :], in_=PS[2][:, :], func=AF.Exp), r=[pk(2)], w=["ebT"])
                    if own:
                        P.ACT(lambda e: e.activation(out=ebT[:, 1, :], in_=PS[2][:, :], func=AF.Exp, scale=-1.0), r=[pk(2)], w=["ebTn"])
                    for k in range(KD):
                        P.PE(lambda e, k=k: e.matmul(PS[3][:, :], hT[:, k, :], WA[:, k, WA_GK:WA_GK + 512], start=(k == 0), stop=(k == KD - 1)),
                             r=["WA"] + HK, w=[pk(3)])
                    P.DVE(lambda e: e.tensor_tensor(out=kp_tok[:], in0=PS[3][:, :], in1=ek[:], op=ALU.mult), r=[pk(3), "ek"], w=["kp_tok"])
                    for hh in range(2):
                        for k in range(KD):
                            P.PE(lambda e, k=k, hh=hh: e.matmul(PS[6 + hh][:, :], hT[:, k, :], WA[:, k, WA_GV + hh * 512:WA_GV + (hh + 1) * 512],
                                                                 start=(k == 0), stop=(k == KD - 1)), r=["WA"] + HK, w=[pk(6 + hh)])
                        P.ACT(lambda e, hh=hh: e.activation(out=v_tok[:, hh * 512:(hh + 1) * 512], in_=PS[6 + hh][:, :], func=AF.Copy,
                                                           scale=valid_sb[:, t:t + 1]), r=[pk(6 + hh), "valid"], w=["v_tok"])
                    if own:
                        for hd in range(4):
                            for k in range(KD):
                                P.PE(lambda e, k=k, hd=hd: e.matmul(PS[0][:, hd * 128:(hd + 1) * 128], WA[:, k, WA_GQ + hd * 128:WA_GQ + (hd + 1) * 128],
                                                                     hT[:, k, :], start=(k == 0), stop=(k == KD - 1)), r=["WA"] + HK, w=[pk(0)])
                        for hd in range(4):
                            for k in range(KD):
                                P.PE(lambda e, k=k, hd=hd: e.matmul(PS[1][:, hd * 128:(hd + 1) * 128], WA[:, k, WA_GK + hd * 128:WA_GK + (hd + 1) * 128],
                                                                     hT[:, k, :], start=(k == 0), stop=(k == KD - 1)), r=["WA"] + HK, w=[pk(1)])
                        P.DVE(lambda e: e.scalar_tensor_tensor(out=qpT[:], in0=PS[0][:, :], scalar=128.0 ** -0.5, in1=ebT[:, 0, :],
                                                               op0=ALU.mult, op1=ALU.mult), r=[pk(0), "ebT"], w=["qpT"])
                        P.DVE(lambda e: e.tensor_tensor(out=kppT[:], in0=PS[1][:, :], in1=ebT[:, 1, :], op=ALU.mult),
                              r=[pk(1), "ebTn"], w=["kppT"])
                        for hd in range(4):
                            P.PE(lambda e, hd=hd: e.matmul(PS[0][:, hd * 128:(hd + 1) * 128], kppT[:, hd * 128:(hd + 1) * 128],
                                                           qpT[:, hd * 128:(hd + 1) * 128], start=True, stop=True), r=["kppT", "qpT"], w=[pk(0)])
                        P.DVE(lambda e: e.tensor_tensor(out=AT[:].rearrange("p (h t) -> p h t", h=4), in0=PS[0][:, :].rearrange("p (h t) -> p h t", h=4),
                                                        in1=cmask[:, 0:128].unsqueeze(1).to_broadcast([128, 4, 128]), op=ALU.mult),
                              r=[pk(0), "cmask"], w=["AT"])
                        for hd in range(4):
                            bank = hd // 2
                            oc = (hd % 2) * 256
                            P.PE(lambda e, hd=hd, bank=bank, oc=oc: e.matmul(PS[bank][:, oc:oc + 256], qpT[:, hd * 128:(hd + 1) * 128],
                                                                           Sbf[:, hd * 256:(hd + 1) * 256], start=True, stop=False),
                                 r=["qpT", "Sbf"], w=[pk(bank)])
                            P.PE(lambda e, hd=hd, bank=bank, oc=oc: e.matmul(PS[bank][:, oc:oc + 256], AT[:, hd * 128:(hd + 1) * 128],
                                                                           v_tok[:, hd * 256:(hd + 1) * 256], start=False, stop=True),
                                 r=["AT", "v_tok"], w=[pk(bank)])
                        tt = t - OWN0
                        for hd in range(4):
                            bank = hd // 2
                            oc = (hd % 2) * 256
                            P.ACT(lambda e, hd=hd, bank=bank, oc=oc: e.activation(out=otmp[:, hd * 256:(hd + 1) * 256], in_=PS[bank][:, oc:oc + 256],
                                                                                func=AF.Square, accum_out=ostat[:, hd:hd + 1]),
                                  r=[pk(bank)], w=["otmp", "ostat"])
                        P.ACT(lambda e: e.activation(out=ostat[:, 4:8], in_=ostat[:, 0:4], func=AF.Sqrt, bias=eps_t[:], scale=1.0 / 256),
                              r=["ostat", "eps_t"], w=["ostat"])
                        P.DVE(lambda e: e.reciprocal(out=ostat[:, 8:12], in_=ostat[:, 4:8]), r=["ostat"], w=["ostat"])
                        for hd in range(4):
                            bank = hd // 2
                            oc = (hd % 2) * 256
                            P.DVE(lambda e, hd=hd, bank=bank, oc=oc: e.scalar_tensor_tensor(
                                out=o_n[:, tt, hd * 256:(hd + 1) * 256], in0=PS[bank][:, oc:oc + 256], scalar=ostat[:, 8 + hd:9 + hd],
                                in1=glanw[:], op0=ALU.mult, op1=ALU.mult), r=[pk(bank), "ostat", "glanw"], w=[("o_n", tt)])
                    for hd in range(4):
                        bank = 6 + hd // 2
                        oc = (hd % 2) * 256
                        P.PE(lambda e, hd=hd, bank=bank, oc=oc: e.matmul(PS[bank][:, oc:oc + 256], kp_tok[:, hd * 128:(hd + 1) * 128],
                                                                       v_tok[:, hd * 256:(hd + 1) * 256], start=True, stop=True),
                             r=["kp_tok", "v_tok"], w=[pk(bank)])
                    for hd in range(4):
                        bank = 6 + hd // 2
                        oc = (hd % 2) * 256
                        P.DVE(lambda e, hd=hd, bank=bank, oc=oc: e.scalar_tensor_tensor(
                            out=Sst[:, hd * 256:(hd + 1) * 256], in0=Sst[:, hd * 256:(hd + 1) * 256],
                            scalar=ebT[:, 0, hd * 128 + 127:hd * 128 + 128], in1=PS[bank][:, oc:oc + 256], op0=ALU.mult, op1=ALU.add),
                            r=[pk(bank), "ebT", "Sst"], w=["Sst"])
                    if t >= OWN0 - 1:
                        P.ACT(lambda e: e.activation(func=AF.Copy, out=Sbf[:], in_=Sst[:]), r=["Sst"], w=["Sbf"])
                    if DEBUG and t == OWN0 - 1:
                        dump("d_S", Sst[:], "Sst", 1024)
                dump("d_ckvn", ckvn[:].rearrange("p c s -> p (c s)"), ("ckvn", 0), 2 * S)
                dump("d_krot", krot[0:64, :], ("krot", 0), S)
                dump("d_on", o_n[:].rearrange("p a b -> p (a b)"), ("o_n", 0), 8192)
                P.barrier()

            if STOP <= 1:
                mix.close()
                return
            mixB = ExitStack()
            glaoT = sb("glaoT", [128, 8, 1024], BF16, mixB)
            qnT = sb("qnT", [128, 8, 1024], BF16, mixB)
            qrotT = sb("qrotT", [128, 8, 1024], BF16, mixB)
            SC = 192.0 ** -0.5
            P.DVE(lambda e: e.memset(qrotT[64:128, :, :], 0.0), w=["qrotpad"])
            with ExitStack() as ph:
                WB = sb("WB", [128, KD, 1536], BF16, ph)
                wqb = sb("wqb", [128, 4, 1536], BF16, ph)
                xt = sb("xt2", [128, D], F32, ph)
                hT = sb("hT2", [128, KD, 128], BF16, ph)
                junk = sb("junk2", [128, D], BF16, ph)
                statt = sb("statt2", [128, 4], F32, ph)
                stat = {"junk": junk, "t": statt}
                sg = sb("sg", [128, 1024], F32, ph)
                gtok = sb("gtok", [128, 1024], F32, ph)
                qlf = sb("qlf", [128, 512], F32, ph)
                sq2 = sb("sq2", [128, 512], BF16, ph)
                rv = sb("rv", [128, 2, 128], F32, ph)
                qlatn = sb("qlatn", [128, 4, 128], BF16, ph)
                rt = sb("rt", [64, 2, 512], F32, ph)
                for (a, n, c0) in [(0, 512, C_GOUT), (512, 512, C_GOUT + 512), (1024, 512, C_QLAT)]:
                    P.dma("pool", WB[:, :, a:a + n], w_in[:, c0:c0 + n].rearrange("(k p) n -> p k n", p=128), w=["WB"], semkey="WB")
                P.finalize("WB")
                P.dma("pool", wqb[:], w_q_b[:, :].rearrange("(k p) n -> p k n", p=128), w=["wqb"])
                HK2 = [("hT2", c) for c in range(KD)]
                for tt in range(8):
                    tsl = slice(tt * 128, (tt + 1) * 128)
                    P.dma("sp", xt[:], xs[(OWN0 + tt) * 128:(OWN0 + tt + 1) * 128, :], w=["xt2"])
                    make_hT(xt[:], "xt2", lambda c: hT[:, c, :], HK2, A_m, B_m, stat)
                    for hh in range(2):
                        for k in range(KD):
                            P.PE(lambda e, k=k, hh=hh: e.matmul(PS[6 + hh][:, :], hT[:, k, :], WB[:, k, hh * 512:(hh + 1) * 512],
                                                                 start=(k == 0), stop=(k == KD - 1)), r=["WB", HK2[k]], w=[pk(6 + hh)])
                        P.ACT(lambda e, hh=hh: e.activation(out=sg[:, hh * 512:(hh + 1) * 512], in_=PS[6 + hh][:, :], func=AF.Silu),
                              r=[pk(6 + hh)], w=["sg"])
                    P.DVE(lambda e, tt=tt: e.tensor_tensor(out=gtok[:], in0=sg[:], in1=o_n[:, tt, :], op=ALU.mult),
                          r=["sg", ("o_n", tt)], w=["gtok"])
                    for g2 in range(2):
                        for i in range(4):
                            c = g2 * 4 + i
                            P.PE(lambda e, c=c, i=i, g2=g2: e.transpose(PS[6 + g2][:, i * 128:(i + 1) * 128], gtok[:, c * 128:(c + 1) * 128], ident[:]),
                                 r=["gtok", "ident"], w=[pk(6 + g2)])
                        P.ACT(lambda e, g2=g2, tsl=tsl: e.activation(out=glaoT[:, g2 * 4:(g2 + 1) * 4, tsl],
                                                                    in_=PS[6 + g2][:, :].rearrange("p (c t) -> p c t", c=4), func=AF.Copy),
                              r=[pk(6 + g2)], w=[("glaoT", tt)])
                    for c in range(4):
                        for k in range(KD):
                            P.PE(lambda e, k=k, c=c: e.matmul(PS[0][:, c * 128:(c + 1) * 128], WB[:, k, 1024 + c * 128:1024 + (c + 1) * 128], hT[:, k, :],
                                                               start=(k == 0), stop=(k == KD - 1)), r=["WB", HK2[k]], w=[pk(0)])
                    P.ACT(lambda e: e.activation(out=qlf[:], in_=PS[0][:, :], func=AF.Copy), r=[pk(0)], w=["qlf"])
                    P.ACT(lambda e: e.activation(out=sq2[:], in_=PS[0][:, :], func=AF.Square), r=[pk(0)], w=["sq2"])
                    for c in range(4):
                        P.PE(lambda e, c=c: e.matmul(PS[1][:, 0:128], ones_b[:], sq2[:, c * 128:(c + 1) * 128], start=(c == 0), stop=(c == 3)),
                             r=["ones_b", "sq2"], w=[pk(1)])
                    P.ACT(lambda e: e.activation(out=rv[:, 0, :], in_=PS[1][:, 0:128], func=AF.Sqrt, bias=eps_t[:], scale=1.0 / 512),
                          r=[pk(1), "eps_t"], w=["rv"])
                    P.DVE(lambda e: e.reciprocal(out=rv[:, 1, :], in_=rv[:, 0, :]), r=["rv"], w=["rv"])
                    for c in range(4):
                        P.DVE(lambda e, c=c: e.scalar_tensor_tensor(out=qlatn[:, c, :], in0=qlf[:, c * 128:(c + 1) * 128], scalar=qnw[:, c:c + 1],
                                                                   in1=rv[:, 1, :], op0=ALU.mult, op1=ALU.mult), r=["qlf", "rv", "vT"], w=["qlatn"])
                    for g2 in range(2):
                        for hl in range(4):
                            hd = g2 * 4 + hl
                            for c in range(4):
                                P.PE(lambda e, c=c, hd=hd, hl=hl, g2=g2: e.matmul(PS[2 + g2][:, hl * 128:(hl + 1) * 128], wqb[:, c, hd * 192:hd * 192 + 128],
                                                                                 qlatn[:, c, :], start=(c == 0), stop=(c == 3)),
                                     r=["wqb", "qlatn"], w=[pk(2 + g2)])
                        P.ACT(lambda e, g2=g2, tsl=tsl: e.activation(out=qnT[:, g2 * 4:(g2 + 1) * 4, tsl],
                                                                    in_=PS[2 + g2][:, :].rearrange("p (c t) -> p c t", c=4), func=AF.Copy, scale=SC),
                              r=[pk(2 + g2)], w=[("qnT", tt)])
                    for v_ in range(2):
                        for g2 in range(2):
                            bank = (0 if v_ == 0 else 4) + g2
                            for hl in range(4):
                                hd = g2 * 4 + hl
                                base = hd * 192 + 128
                                for half in range(2):
                                    par = half if v_ == 0 else 1 - half
                                    for c in range(4):
                                        P.PE(lambda e, c=c, bank=bank, half=half, hl=hl, base=base, par=par: e.matmul(
                                            PS[bank][half * 32:(half + 1) * 32, hl * 128:(hl + 1) * 128], wqb[:, c, base + par:base + 64:2],
                                            qlatn[:, c, :], start=(c == 0), stop=(c == 3)), r=["wqb", "qlatn"], w=[pk(bank)])
                    for g2 in range(2):
                        cosb = cs_own[:, 0, tsl].unsqueeze(1).to_broadcast([64, 4, 128])
                        sinb = cs_own[:, 1, tsl].unsqueeze(1).to_broadcast([64, 4, 128])
                        v3 = lambda ap: ap.rearrange("p (c t) -> p c t", c=4)
                        P.DVE(lambda e, g2=g2, cosb=cosb: e.tensor_tensor(out=v3(rt[:, 0, :]), in0=v3(PS[g2][0:64, :]), in1=cosb, op=ALU.mult),
                              r=[pk(g2), "cs_own"], w=["rt0"])
                        P.DVE(lambda e, g2=g2, sinb=sinb: e.tensor_tensor(out=v3(rt[:, 1, :]), in0=v3(PS[4 + g2][0:64, :]), in1=sinb, op=ALU.mult),
                              r=[pk(4 + g2), "cs_own"], w=["rt1"])
                        P.DVE(lambda e: e.tensor_tensor(out=rt[:, 0, :], in0=rt[:, 0, :], in1=rt[:, 1, :], op=ALU.add), r=["rt0", "rt1"], w=["rt0"])
                        P.ACT(lambda e, g2=g2, tsl=tsl: e.activation(out=qrotT[0:64, g2 * 4:(g2 + 1) * 4, tsl], in_=v3(rt[:, 0, :]), func=AF.Copy, scale=SC),
                              r=["rt0"], w=[("qrotT", tt)])
                dump("d_glao", glaoT[:].rearrange("p a b -> p (a b)"), ("glaoT", 7), 8192)
                dump("d_qn", qnT[:].rearrange("p a b -> p (a b)"), ("qnT", 7), 8192)
                dump("d_qrot", qrotT[0:64, :, :].rearrange("p a b -> p (a b)"), ("qrotT", 7), 8192)
                P.barrier()

            if STOP <= 2:
                mixB.close()
                mix.close()
                return
            mixC = ExitStack()
            mlaoT = sb("mlaoT", [128, 8, 1024], BF16, mixC)
            with ExitStack() as ph:
                wkvb = sb("wkvb", [128, 2, 2048], BF16, ph)
                knT = sb("knT", [128, S], BF16, ph)
                Vh = sb("Vh", [128, NT, 128], BF16, ph)
                pT = [sb("pT%d" % i, [128, 512], BF16, ph) for i in range(2)]
                rs = sb("rs", [128, 512], F32, ph)
                P.dma("pool", wkvb[:], w_kv_b[:, :].rearrange("(k p) n -> p k n", p=128), w=["wkvb"])
                for h in range(8):
                    for sblk in range(8):
                        bank = sblk % 2
                        for c in range(2):
                            P.PE(lambda e, c=c, bank=bank, sblk=sblk, h=h: e.matmul(PS[bank][:, :], wkvb[:, c, h * 256:h * 256 + 128],
                                                                                  ckvn[:, c, sblk * 512:(sblk + 1) * 512], start=(c == 0), stop=(c == 1)),
                                 r=["wkvb"] + [("ckvn", t) for t in range(sblk * 4, sblk * 4 + 4)], w=[pk(bank)])
                        if sblk % 2 == 0:
                            P.ACT(lambda e, bank=bank, sblk=sblk: e.activation(out=knT[:, sblk * 512:(sblk + 1) * 512], in_=PS[bank][:, :], func=AF.Copy),
                                  r=[pk(bank)], w=[("knT", sblk)])
                        else:
                            P.DVE(lambda e, bank=bank, sblk=sblk: e.tensor_copy(out=knT[:, sblk * 512:(sblk + 1) * 512], in_=PS[bank][:, :]),
                                  r=[pk(bank)], w=[("knT", sblk)])
                    for g in range(8):
                        bank = 2 + g % 2
                        for i in range(4):
                            t = g * 4 + i
                            for c in range(2):
                                P.PE(lambda e, c=c, bank=bank, i=i, t=t, h=h: e.matmul(PS[bank][:, i * 128:(i + 1) * 128], ckvn[:, c, t * 128:(t + 1) * 128],
                                                                                     wkvb[:, c, h * 256 + 128:h * 256 + 256], start=(c == 0), stop=(c == 1)),
                                     r=["wkvb", ("ckvn", t)], w=[pk(bank)])
                        if g % 2 == 0:
                            P.ACT(lambda e, bank=bank, g=g: e.activation(out=Vh[:, g * 4:(g + 1) * 4, :], in_=PS[bank][:, :].rearrange("p (c t) -> p c t", c=4),
                                                                        func=AF.Copy), r=[pk(bank)], w=[("Vh", g)])
                        else:
                            P.DVE(lambda e, bank=bank, g=g: e.tensor_copy(out=Vh[:, g * 4:(g + 1) * 4, :], in_=PS[bank][:, :].rearrange("p (c t) -> p c t", c=4)),
                                  r=[pk(bank)], w=[("Vh", g)])
                    for qb in range(2):
                        nk = 28 + 4 * qb
                        qs = slice(qb * 512, (qb + 1) * 512)
                        qkeys = [("qnT", t) for t in range(qb * 4, qb * 4 + 4)]
                        qrkeys = [("qrotT", t) for t in range(qb * 4, qb * 4 + 4)]
                        def scores(kt):
                            sbk = kt % 2
                            ks = slice(kt * 128, (kt + 1) * 128)
                            P.PE(lambda e, sbk=sbk, ks=ks, h=h, qs=qs: e.matmul(PS[sbk][:, :], knT[:, ks], qnT[:, h, qs], start=True, stop=False),
                                 r=[("knT", kt // 4)] + qkeys, w=[pk(sbk)])
                            P.PE(lambda e, sbk=sbk, ks=ks, h=h, qs=qs: e.matmul(PS[sbk][:, :], krot[:, ks], qrotT[:, h, qs], start=False, stop=True),
                                 r=[("krot", kt), "krotpad", "qrotpad"] + qrkeys, w=[pk(sbk)])

                        scores(0)
                        for kt in range(nk):
                            sbk = kt % 2
                            if kt + 1 < nk:
                                scores(kt + 1)
                            P.ACT(lambda e, sbk=sbk, kt=kt: e.activation(out=pT[sbk][:], in_=PS[sbk][:, :], func=AF.Exp, bias=kbias[:, kt:kt + 1], scale=1.0),
                                  r=[pk(sbk), "kbias"], w=[("pT", sbk)])
                            m = kt - (24 + 4 * qb)
                            if m >= 0:
                                P.DVE(lambda e, sbk=sbk, m=m: e.tensor_tensor(out=pT[sbk][:], in0=pT[sbk][:], in1=cmask[:, m * 512:(m + 1) * 512], op=ALU.mult),
                                      r=[("pT", sbk), "cmask"], w=[("pT", sbk)])
                            P.PE(lambda e, sbk=sbk, kt=kt, qb=qb, nk=nk: e.matmul(PS[4 + qb][:, :], Vh[:, kt, :], pT[sbk][:], start=(kt == 0), stop=(kt == nk - 1)),
                                 r=[("Vh", kt // 4), ("pT", sbk)], w=[pk(4 + qb)])
                            P.PE(lambda e, sbk=sbk, kt=kt, qb=qb, nk=nk: e.matmul(PS[6 + qb][:, :], ones_b[:], pT[sbk][:], start=(kt == 0), stop=(kt == nk - 1)),
                                 r=["ones_b", ("pT", sbk)], w=[pk(6 + qb)])
                        P.DVE(lambda e, qb=qb: e.reciprocal(out=rs[:], in_=PS[6 + qb][:, :]), r=[pk(6 + qb)], w=["rs"])
                        P.DVE(lambda e, qb=qb, h=h, qs=qs: e.tensor_tensor(out=mlaoT[:, h, qs], in0=PS[4 + qb][:, :], in1=rs[:], op=ALU.mult),
                              r=[pk(4 + qb), "rs"], w=[("mlaoT", h)])
                dump("d_mlao", mlaoT[:].rearrange("p a b -> p (a b)"), ("mlaoT", 7), 8192)
                P.dma("sp", glao_d[:, :], glaoT[:].rearrange("p a b -> p (a b)"), r=[("glaoT", t) for t in range(8)], w=["glao_d"])
                P.dma("sp", mlao_d[:, :], mlaoT[:].rearrange("p a b -> p (a b)"), r=[("mlaoT", t) for t in range(8)], w=["mlao_d"])
                P.barrier()
            P.barrier()
            mixC.close()
            mixB.close()
            mix.close()

            if STOP <= 3:
                return

            def bc_row(dst, dkey, srcT, dg):
                for k in range(KD):
                    P.DVE(lambda e, k=k: e.tensor_scalar(out=dg[:], in0=ident[:], scalar1=srcT[:, k:k + 1], scalar2=None, op0=ALU.mult),
                          r=["ident", "modT", "AB"], w=["dg"])
                    bank = (k // 4) % 2
                    P.PE(lambda e, k=k, bank=bank: e.matmul(PS[bank][:, (k % 4) * 128:(k % 4 + 1) * 128], ones_f[:], dg[:], start=True, stop=True),
                         r=["ones_f", "dg"], w=[pk(bank)])
                    if k % 4 == 3:
                        P.ACT(lambda e, k=k, bank=bank: e.activation(out=dst[:, (k // 4) * 512:(k // 4 + 1) * 512], in_=PS[bank][:, :], func=AF.Copy),
                              r=[pk(bank)], w=[dkey])

            ffn = ExitStack()
            h2T = sb("h2T", [128, KD, 1024], BF16, ffn)
            with ExitStack() as ph4:
                mergedT = sb("mergedT", [128, KD, 1024], BF16, ph4)
                with ExitStack() as ph:
                    glaoT2 = sb("glaoT2", [128, 8, 1024], BF16, ph)
                    mlaoT2 = sb("mlaoT2", [128, 8, 1024], BF16, ph)
                    hTo = sb("hTo", [128, KD, 1024], BF16, ph)
                    xt = sb("xt4", [128, D], F32, ph)
                    junk = sb("junk4", [128, D], BF16, ph)
                    statt = sb("statt4", [128, 4], F32, ph)
                    stat = {"junk": junk, "t": statt}
                    wga = sb("wga", [128, KD, 512], BF16, ph)
                    wgb = sb("wgb", [128, KD, 512], BF16, ph)
                    woa = sb("woa", [128, 8, 512], BF16, ph)
                    wob = sb("wob", [128, 8, 512], BF16, ph)
                    sa_ = sb("sa_", [128, 512], F32, ph)
                    sb_ = sb("sb_", [128, 512], F32, ph)
                    P.dma("sp", glaoT2[:].rearrange("p a b -> p (a b)"), glao_d[:, :], r=["glao_d"], w=["glaoT2"])
                    P.dma("sp", mlaoT2[:].rearrange("p a b -> p (a b)"), mlao_d[:, :], r=["mlao_d"], w=["mlaoT2"])
                    for tt in range(8):
                        P.dma("sp", xt[:], xs[(OWN0 + tt) * 128:(OWN0 + tt + 1) * 128, :], w=["xt4"])
                        make_hT(xt[:], "xt4", lambda c, tt=tt: hTo[:, c, tt * 128:(tt + 1) * 128], [("hTo", tt, c) for c in range(KD)], A_m, B_m, stat)
                    for dcg in range(4):
                        cs_ = slice(dcg * 512, (dcg + 1) * 512)
                        P.dma("pool", wga[:], w_in[:, C_GA + dcg * 512:C_GA + (dcg + 1) * 512].rearrange("(k p) n -> p k n", p=128), w=["wga"])
                        P.dma("pool", wgb[:], w_in[:, C_GB + dcg * 512:C_GB + (dcg + 1) * 512].rearrange("(k p) n -> p k n", p=128), w=["wgb"])
                        P.dma("pool", woa[:], w_o_mla[:, cs_].rearrange("(k p) n -> p k n", p=128), w=["woa"])
                        P.dma("pool", wob[:], w_o_gla[:, cs_].rearrange("(k p) n -> p k n", p=128), w=["wob"])
                        for dcl in range(4):
                            dc = dcg * 4 + dcl
                            ws = slice(dcl * 128, (dcl + 1) * 128)
                            for th in range(2):
                                ts_ = slice(th * 512, (th + 1) * 512)
                                for k in range(KD):
                                    P.PE(lambda e, k=k, ws=ws, ts_=ts_: e.matmul(PS[0][:, :], wga[:, k, ws], hTo[:, k, ts_], start=(k == 0), stop=(k == KD - 1)),
                                         r=["wga"] + [("hTo", t, k) for t in range(th * 4, th * 4 + 4)], w=[pk(0)])
                                for k in range(KD):
                                    P.PE(lambda e, k=k, ws=ws, ts_=ts_: e.matmul(PS[1][:, :], wgb[:, k, ws], hTo[:, k, ts_], start=(k == 0), stop=(k == KD - 1)),
                                         r=["wgb"] + [("hTo", t, k) for t in range(th * 4, th * 4 + 4)], w=[pk(1)])
                                for c in range(8):
                                    P.PE(lambda e, c=c, ws=ws, ts_=ts_: e.matmul(PS[2][:, :], woa[:, c, ws], mlaoT2[:, c, ts_], start=(c == 0), stop=(c == 7)),
                                         r=["woa", "mlaoT2"], w=[pk(2)])
                                for c in range(8):
                                    P.PE(lambda e, c=c, ws=ws, ts_=ts_: e.matmul(PS[3][:, :], wob[:, c, ws], glaoT2[:, c, ts_], start=(c == 0), stop=(c == 7)),
                                         r=["wob", "glaoT2"], w=[pk(3)])
                                P.ACT(lambda e: e.activation(out=sa_[:], in_=PS[0][:, :], func=AF.Sigmoid), r=[pk(0)], w=["sa_"])
                                P.ACT(lambda e: e.activation(out=sb_[:], in_=PS[1][:, :], func=AF.Sigmoid), r=[pk(1)], w=["sb_"])
                                P.DVE(lambda e: e.tensor_tensor(out=sa_[:], in0=sa_[:], in1=PS[2][:, :], op=ALU.mult), r=["sa_", pk(2)], w=["sa_"])
                                P.DVE(lambda e: e.tensor_tensor(out=sb_[:], in0=sb_[:], in1=PS[3][:, :], op=ALU.mult), r=["sb_", pk(3)], w=["sb_"])
                                P.DVE(lambda e, dc=dc, ts_=ts_: e.tensor_tensor(out=mergedT[:, dc, ts_], in0=sa_[:], in1=sb_[:], op=ALU.add),
                                      r=["sa_", "sb_"], w=[("mergedT", dc, th)])
                    dump("d_merged", mergedT[:].rearrange("p a b -> p (a b)"), ("mergedT", 15, 1), 16384)
                    P.barrier()
                with ExitStack() as ph:
                    wout = sb("wout", [128, KD, D], BF16, ph)
                    wr = sb("wr", [128, KD, NE], F32, ph)
                    gmbc = sb("gmbc", [128, D], F32, ph)
                    dg = sb("dg", [128, 128], F32, ph)
                    xt = sb("xt4b", [128, D], F32, ph)
                    x1t = sb("x1t", [128, D], F32, ph)
                    h2f = sb("h2f", [128, KD, 128], F32, ph)
                    junk = sb("junk4b", [128, D], BF16, ph)
                    statt = sb("statt4b", [128, 4], F32, ph)
                    stat = {"junk": junk, "t": statt}
                    gt = sb("gt", [128, 4, NE], F32, ph)
                    gs8 = sb("gs8", [128, 16], F32, ph)
                    for q4 in range(4):
                        P.dma("pool", wout[:, :, q4 * 512:(q4 + 1) * 512], w_out[:, q4 * 512:(q4 + 1) * 512].rearrange("(k p) n -> p k n", p=128),
                              w=["wout"], semkey="wout")
                    P.finalize("wout")
                    P.dma("sp", wr[:], w_router[:, :].rearrange("(k p) n -> p k n", p=128), w=["wr"])
                    bc_row(gmbc, "gmbc", modT[:, 32:48], dg)
                    for tt in range(8):
                        tsl = slice(tt * 128, (tt + 1) * 128)
                        P.dma("sp", xt[:], xs[(OWN0 + tt) * 128:(OWN0 + tt + 1) * 128, :], w=["xt4b"])
                        for dt_ in range(4):
                            bank = dt_ % 2
                            ds_ = slice(dt_ * 512, (dt_ + 1) * 512)
                            for k in range(KD):
                                P.PE(lambda e, k=k, bank=bank, tsl=tsl, ds_=ds_: e.matmul(PS[bank][:, :], mergedT[:, k, tsl], wout[:, k, ds_],
                                                                                         start=(k == 0), stop=(k == KD - 1)),
                                     r=["wout", ("mergedT", k, tt // 4)], w=[pk(bank)])
                            P.DVE(lambda e, bank=bank, ds_=ds_: e.tensor_tensor(out=x1t[:, ds_], in0=PS[bank][:, :], in1=gmbc[:, ds_], op=ALU.mult),
                                  r=[pk(bank), "gmbc"], w=["x1t"])
                            P.DVE(lambda e, ds_=ds_: e.tensor_tensor(out=x1t[:, ds_], in0=x1t[:, ds_], in1=xt[:, ds_], op=ALU.add),
                                  r=["x1t", "xt4b"], w=["x1t"])
                        P.dma("sp", x1d[tsl, :], x1t[:], r=["x1t"], w=[("x1d", tt)], semkey="x1st")
                        if DEBUG:
                            P.dma("pool", dbg["d_x1"][:, tt * 2048:(tt + 1) * 2048], x1t[:], r=["x1t"], semkey=("dbg", "d_x1"))
                        make_hT(x1t[:], "x1t", lambda c, tsl=tsl: h2T[:, c, tsl], [("h2T", tt, c) for c in range(KD)], A_f, B_f, stat,
                                f32dst_fn=lambda c: h2f[:, c, :], f32keys=[("h2f", c) for c in range(KD)])
                        for k in range(KD):
                            P.PE(lambda e, k=k: e.matmul(PS[2][:, 0:NE], h2f[:, k, :], wr[:, k, :], start=(k == 0), stop=(k == KD - 1)),
                                 r=[("h2f", k), "wr"], w=[pk(2)])
                        lg, msk, ex = gt[:, 0, :], gt[:, 1, :], gt[:, 2, :]
                        P.DVE(lambda e: e.tensor_tensor(out=lg, in0=PS[2][:, 0:NE], in1=brout[:], op=ALU.add), r=[pk(2), "brout"], w=["lg"])
                        P.DVE(lambda e: e.max(out=gs8[:, 0:8], in_=lg), r=["lg"], w=["mx8"])
                        P.DVE(lambda e: e.tensor_scalar(out=msk, in0=lg, scalar1=gs8[:, 3:4], scalar2=None, op0=ALU.is_ge), r=["lg", "mx8"], w=["msk"])
                        P.DVE(lambda e: e.tensor_scalar(out=gs8[:, 8:9], in0=gs8[:, 0:1], scalar1=-1.0, scalar2=None, op0=ALU.mult), r=["mx8"], w=["nmx"])
                        P.ACT(lambda e: e.activation(out=ex, in_=lg, func=AF.Exp, bias=gs8[:, 8:9], scale=1.0), r=["lg", "nmx"], w=["ex"])
                        P.DVE(lambda e: e.tensor_tensor(out=ex, in0=ex, in1=msk, op=ALU.mult), r=["ex", "msk"], w=["ex"])
                        P.DVE(lambda e: e.tensor_reduce(out=gs8[:, 9:10], in_=ex, axis=AX.X, op=ALU.add), r=["ex"], w=["ssum"])
                        P.DVE(lambda e: e.reciprocal(out=gs8[:, 10:11], in_=gs8[:, 9:10]), r=["ssum"], w=["rsum"])
                        P.DVE(lambda e, tt=tt: e.tensor_scalar(out=G[:, tt, :], in0=ex, scalar1=gs8[:, 10:11], scalar2=None, op0=ALU.mult),
                              r=["ex", "rsum"], w=[("G", tt)])
                    P.finalize("x1st")
                    dump("d_G", G[:].rearrange("p a b -> p (a b)"), ("G", 7), 256)
                    P.barrier()
            P.barrier()

            if STOP <= 4:
                ffn.close()
                return
            moe = ExitStack()
            acc = sb("acc", [128, 8, D], F32, moe)
            b1T = sb("b1T", [128, 32, NE], F32, moe)
            gTb = sb("gTb", [32, 8, 128], BF16, moe)
            b2b = sb("b2b", [32, D], BF16, moe)
            with ExitStack() as ph:
                b1s = sb("b1s", [32, 2 * D], F32, ph)
                P.dma("sp", b1s[:], b1[:, :], w=["b1s"])
                for c2 in range(32):
                    c, par = c2 // 2, c2 % 2
                    bank = c2 // 16
                    P.PE(lambda e, c=c, par=par, bank=bank, c2=c2: e.transpose(PS[bank][:, (c2 % 16) * 32:(c2 % 16 + 1) * 32],
                                                                             b1s[:, 256 * c + par:256 * c + 256:2], ident[0:32, 0:32]),
                         r=["b1s", "ident"], w=[pk(bank)])
                for bank in range(2):
                    P.ACT(lambda e, bank=bank: e.activation(out=b1T[:, bank * 16:(bank + 1) * 16, :], in_=PS[bank][:, :].rearrange("p (c e) -> p c e", c=16),
                                                           func=AF.Copy), r=[pk(bank)], w=["b1T"])
                P.barrier()
            P.dma("pool", b2b[:], b2[:, :], w=["b2b"])
            for tt in range(8):
                bank = 2 + tt // 4
                P.PE(lambda e, tt=tt, bank=bank: e.transpose(PS[bank][0:32, (tt % 4) * 128:(tt % 4 + 1) * 128], G[:, tt, :], ident[:]),
                     r=[("G", tt), "ident"], w=[pk(bank)])
            for bank in range(2):
                P.ACT(lambda e, bank=bank: e.activation(out=gTb[:, bank * 4:(bank + 1) * 4, :], in_=PS[2 + bank][0:32, :].rearrange("p (c t) -> p c t", c=4),
                                                       func=AF.Copy), r=[pk(2 + bank)], w=["gTb"])
            for tt in range(8):
                for dt_ in range(4):
                    bank = 4 + dt_ % 2
                    ds_ = slice(dt_ * 512, (dt_ + 1) * 512)
                    P.PE(lambda e, tt=tt, bank=bank, ds_=ds_: e.matmul(PS[bank][:, :], gTb[:, tt, :], b2b[:, ds_], start=True, stop=True),
                         r=["gTb", "b2b"], w=[pk(bank)])
                    P.ACT(lambda e, tt=tt, bank=bank, ds_=ds_: e.activation(out=acc[:, tt, ds_], in_=PS[bank][:, :], func=AF.Copy),
                          r=[pk(bank)], w=[("acc", tt, dt_)])
            with ExitStack() as ph:
                actT = sb("actT", [128, KD, 1024], BF16, ph)
                w1b = [sb("w1b%d" % i, [128, KD, 256], BF16, ph) for i in range(2)]
                w2b = [sb("w2b%d" % i, [128, KD, 512], BF16, ph) for i in range(2)]
                tg = sb("tg", [128, 512], F32, ph)
                tsg = sb("tsg", [128, 512], F32, ph)
                tl = sb("tl", [128, 512], F32, ph)
                NX = N_EXPERTS_RUN
                L1 = [(e_, c) for e_ in range(NX) for c in range(16)]
                L2 = [(e_, d_) for e_ in range(NX) for d_ in range(4)]

                def load1(i):
                    e_, c = L1[i]
                    P.dma("pool", w1b[i % 2][:], w1[e_, :, 256 * c:256 * (c + 1)].rearrange("(k p) n -> p k n", p=128), w=[("w1b", i % 2)])

                def load2(i):
                    e_, d_ = L2[i]
                    P.dma("pool", w2b[i % 2][:], w2[e_, :, 512 * d_:512 * (d_ + 1)].rearrange("(k p) n -> p k n", p=128), w=[("w2b", i % 2)])

                load1(0)
                load2(0)
                for e_ in range(NX):
                    for c in range(16):
                        i1 = e_ * 16 + c
                        if i1 + 1 < len(L1):
                            load1(i1 + 1)
                        wb = w1b[i1 % 2]
                        wkey = ("w1b", i1 % 2)
                        for th in range(2):
                            ts_ = slice(th * 512, (th + 1) * 512)
                            pg, pl = 2 * th, 2 * th + 1
                            for par, bank in ((0, pg), (1, pl)):
                                for k in range(KD):
                                    P.PE(lambda e, k=k, par=par, bank=bank, wb=wb, ts_=ts_: e.matmul(PS[bank][:, :], wb[:, k, par:256:2], h2T[:, k, ts_],
                                                                                                start=(k == 0), stop=(k == KD - 1)),
                                         r=[wkey] + [("h2T", t, k) for t in range(th * 4, th * 4 + 4)], w=[pk(bank)])
                            P.DVE(lambda e, pg=pg, c=c, e_=e_: e.tensor_scalar(out=tg[:], in0=PS[pg][:, :], scalar1=b1T[:, 2 * c, e_:e_ + 1], scalar2=7.0,
                                                                             op0=ALU.add, op1=ALU.min), r=[pk(pg), "b1T"], w=["tg"])
                            P.ACT(lambda e: e.activation(out=tsg[:], in_=tg[:], func=AF.Sigmoid, scale=1.702), r=["tg"], w=["tsg"])
                            P.DVE(lambda e, pl=pl, c=c, e_=e_: e.tensor_scalar(out=tl[:], in0=PS[pl][:, :], scalar1=b1T[:, 2 * c + 1, e_:e_ + 1], scalar2=7.0,
                                                                             op0=ALU.add, op1=ALU.min), r=[pk(pl), "b1T"], w=["tl"])
                            P.DVE(lambda e: e.tensor_scalar(out=tl[:], in0=tl[:], scalar1=-7.0, scalar2=1.0, op0=ALU.max, op1=ALU.add), r=["tl"], w=["tl"])
                            P.DVE(lambda e: e.tensor_tensor(out=tg[:], in0=tg[:], in1=tsg[:], op=ALU.mult), r=["tg", "tsg"], w=["tg"])
                            P.DVE(lambda e, c=c, ts_=ts_: e.tensor_tensor(out=actT[:, c, ts_], in0=tg[:], in1=tl[:], op=ALU.mult),
                                  r=["tg", "tl"], w=[("actT", c, th)])
                    for d_ in range(4):
                        i2 = e_ * 4 + d_
                        if i2 + 1 < len(L2):
                            load2(i2 + 1)
                        wb = w2b[i2 % 2]
                        wkey = ("w2b", i2 % 2)
                        ds_ = slice(d_ * 512, (d_ + 1) * 512)
                        for tt in range(8):
                            bank = 4 + tt % 2
                            tsl = slice(tt * 128, (tt + 1) * 128)
                            for c in range(KD):
                                P.PE(lambda e, c=c, bank=bank, wb=wb, tsl=tsl: e.matmul(PS[bank][:, :], actT[:, c, tsl], wb[:, c, :],
                                                                                      start=(c == 0), stop=(c == KD - 1)),
                                     r=[wkey, ("actT", c, tt // 4)], w=[pk(bank)])
                            P.DVE(lambda e, bank=bank, tt=tt, ds_=ds_, e_=e_: e.scalar_tensor_tensor(out=acc[:, tt, ds_], in0=PS[bank][:, :],
                                                                                                  scalar=G[:, tt, e_:e_ + 1], in1=acc[:, tt, ds_],
                                                                                                  op0=ALU.mult, op1=ALU.add),
                                  r=[pk(bank), ("G", tt), ("acc", tt, d_)], w=[("acc", tt, d_)])
                P.barrier()
            if DEBUG:
                for tt in range(8):
                    P.dma("pool", dbg["d_acc"][:, tt * 2048:(tt + 1) * 2048], acc[:, tt, :], r=[("acc", tt, d_) for d_ in range(4)], semkey=("dbg", "d_acc"))

            with ExitStack() as ph:
                gfbc = sb("gfbc", [128, D], F32, ph)
                aobc = sb("aobc", [128, D], F32, ph)
                bobc = sb("bobc", [128, D], F32, ph)
                dg = sb("dg7", [128, 128], F32, ph)
                x1t = sb("x1t7", [128, D], F32, ph)
                yt = [sb("yt%d" % i, [128, D], F32, ph) for i in range(2)]
                junk = sb("junk7", [128, D], BF16, ph)
                st7 = sb("st7", [128, 4], F32, ph)
                bc_row(gfbc, "gfbc", modT[:, 80:96], dg)
                bc_row(aobc, "aobc", A_o, dg)
                bc_row(bobc, "bobc", B_o, dg)
                for tt in range(8):
                    tsl = slice(tt * 128, (tt + 1) * 128)
                    y = yt[tt % 2]
                    yk = ("yt", tt % 2)
                    P.dma("sp", x1t[:], x1d[tsl, :], r=[("x1d", tt)], w=["x1t7"])
                    P.DVE(lambda e, tt=tt, y=y: e.tensor_tensor(out=y[:], in0=acc[:, tt, :], in1=gfbc[:], op=ALU.mult),
                          r=[("acc", tt, d_) for d_ in range(4)] + ["gfbc"], w=[yk])
                    P.DVE(lambda e, y=y: e.tensor_tensor(out=y[:], in0=y[:], in1=x1t[:], op=ALU.add), r=[yk, "x1t7"], w=[yk])
                    P.ACT(lambda e, y=y: e.activation(out=junk[:], in_=y[:], func=AF.Square, accum_out=st7[:, 0:1]), r=[yk], w=["junk7", "st7"])
                    P.ACT(lambda e: e.activation(out=st7[:, 1:2], in_=st7[:, 0:1], func=AF.Sqrt, bias=eps_t[:], scale=1.0 / D), r=["st7", "eps_t"], w=["st7"])
                    P.DVE(lambda e: e.reciprocal(out=st7[:, 2:3], in_=st7[:, 1:2]), r=["st7"], w=["st7"])
                    P.DVE(lambda e, y=y: e.scalar_tensor_tensor(out=y[:], in0=y[:], scalar=st7[:, 2:3], in1=aobc[:], op0=ALU.mult, op1=ALU.mult),
                          r=[yk, "st7", "aobc"], w=[yk])
                    P.DVE(lambda e, y=y: e.tensor_tensor(out=y[:], in0=y[:], in1=bobc[:], op=ALU.add), r=[yk, "bobc"], w=[yk])
                    P.dma("sp", out[tsl, :], y[:], r=[yk], w=[("out", tt)], semkey="outst")
                P.barrier()
            moe.close()
            ffn.close()

        block.sync(body)
        P.final_wait()
    return nc


def _consts():
    ident = np.eye(128, dtype=np.float32)
    tp = np.arange(128)[:, None]
    tt = np.arange(128)[None, :]
    tri = np.where(tp <= tt, -1.0 / 16, 0.0).astype(np.float32)
    suf = np.where(tp > tt, -1.0 / 16, 0.0).astype(np.float32)
    c_tri = np.concatenate([tri, suf], axis=1)
    f = np.arange(512)[None, :]
    p = np.arange(128)[:, None]
    c_mask = np.concatenate([(f - p - 128 * m >= 0).astype(np.float32) for m in range(4)], axis=1)
    inv = (10000.0 ** (-(np.arange(0, 64, 2, dtype=np.float32) / 64))).astype(np.float32)
    c_rope = np.zeros((64, 2), np.float32)
    c_rope[:, 0] = np.concatenate([inv, inv])
    c_rope[:, 1] = np.concatenate([-np.ones(32), np.ones(32)])
    return dict(c_ident=ident, c_tri=np.ascontiguousarray(c_tri), c_mask=np.ascontiguousarray(c_mask), c_rope=c_rope)


def _prep_inputs(inp):
    f = lambda a: np.ascontiguousarray(np.asarray(a))
    x = f(inp["x"]); c = f(inp["c"]); pos = f(inp["positions"])
    shared = dict(
        w_ada=f(inp["w_ada"][0]), b_ada=f(inp["b_ada"][0]).reshape(96, 128),
        w_ada_final=f(inp["w_ada_final"]), b_ada_final=f(inp["b_ada_final"]).reshape(32, 128),
        vecs=np.concatenate([f(inp["norm_mix_w"][0]).reshape(16, 128), f(inp["norm_ffn_w"][0]).reshape(16, 128),
                             f(inp["norm_final_w"]).reshape(16, 128), f(inp["mla_q_norm_w"][0]).reshape(4, 128),
                             f(inp["mla_kv_norm_w"][0]).reshape(2, 128)], axis=0),
        w_in=f(inp["w_in"][0]), mla_w_q_b=f(inp["mla_w_q_b"][0]), mla_w_kv_b=f(inp["mla_w_kv_b"][0]),
        wgk1=np.concatenate([f(inp["gla_w_gk_up"][0]), f(inp["gla_b_gk_up"][0]).reshape(1, 512)], axis=0),
        gla_nw=np.ascontiguousarray(np.broadcast_to(f(inp["gla_norm_w"][0]).reshape(1, 256), (128, 256))),
        w_o_mla=f(inp["w_o_mla"][0]), w_o_gla=f(inp["w_o_gla"][0]), w_out=f(inp["w_out"][0]),
        w_router=f(inp["w_router"][0]),
        b_router=np.ascontiguousarray(np.broadcast_to(f(inp["b_router"][0]).reshape(1, NE), (128, NE))),
        w1=f(inp["w1"][0]), b1=f(inp["b1"][0]), w2=f(inp["w2"][0]), b2=f(inp["b2"][0]),
    )
    shared.update(_consts())
    in_maps = []
    for core in range(8):
        b, j = core // 4, core % 4
        npad = (3 - j) * 1024
        xs = np.zeros((S, D), np.float32)
        xs[npad:] = x[b, : (j + 1) * 1024]
        ps = np.zeros((S,), np.int32)
        ps[npad:] = pos[b, : (j + 1) * 1024]
        slot = np.arange(S).reshape(NT, 128).T
        m = dict(shared)
        m["xs"] = xs
        m["posr"] = np.ascontiguousarray(np.broadcast_to(ps[None, :], (64, S)))
        m["valid"] = (slot >= npad).astype(np.float32)
        m["cT"] = np.ascontiguousarray(c[b].reshape(KD, 128).T)
        in_maps.append(m)
    return in_maps


def kernel(**inputs):
    in_maps = _prep_inputs(inputs)
    if STOP <= 4:
        in_maps = [{k: v for k, v in m.items() if k not in ("w1", "b1", "w2", "b2")} for m in in_maps]
    nc = build_nc()
    res = run_bass_kernel_spmd(nc, in_maps, core_ids=list(range(8)))
    if DEBUG:
        kernel.last = res
    outs = [r["out"] for r in res.results]
    full = np.zeros((2, 4096, D), np.float32)
    for core in range(8):
        b, j = core // 4, core % 4
        full[b, j * 1024:(j + 1) * 1024] = outs[core]
    return full
```

```python
import os
from contextlib import ExitStack
import numpy as np
import concourse.bass as bass
import concourse.mybir as mybir
from concourse.bass_utils import run_bass_kernel_spmd

F32 = mybir.dt.float32
BF16 = mybir.dt.bfloat16
I32 = mybir.dt.int32
AF = mybir.ActivationFunctionType
ALU = mybir.AluOpType
AX = mybir.AxisListType

D = 2048
KD = 16
S = 4096
NT = 32
OWN0 = 24
NE = 32
EPS = 1e-6
C_QLAT, C_CKV, C_KPE, C_GQ, C_GK, C_GV, C_GLR, C_GOUT, C_GA, C_GB = 0, 512, 768, 832, 1344, 1856, 2880, 2896, 3920, 5968
WA_CKV, WA_KPE, WA_GK, WA_GV, WA_GLR, WA_GQ, WA_N = 0, 256, 320, 832, 1856, 1872, 2384

DEBUG = os.environ.get("MK_DEBUG", "")
N_EXPERTS_RUN = int(os.environ.get("MK_NEXP", "32"))
STOP = int(os.environ.get("MK_STOP", "99"))


class _S:
    def __init__(self, sem, selfsync=False, handle=None, name=""):
        self.sem = sem
        self.val = 0
        self.selfsync = selfsync
        self.h = handle
        self.seen = {}
        self.name = name


class _Reg:
    __slots__ = ("writers", "readers")

    def __init__(self):
        self.writers = {}
        self.readers = {}


class Prog:
    def __init__(self, nc, es):
        self.nc = nc
        self.es = es
        self.regs = {}
        self.dsems = {}
        self.groups = {}
        mk = lambda n: es.enter_context(nc.semaphore(n))
        self.pe = _S(mk("s_pe"), False, nc.tensor, "pe")
        self.act = _S(mk("s_act"), True, nc.scalar, "act")
        self.dve = _S(mk("s_dve"), True, nc.vector, "dve")
        self.pool = _S(mk("s_pool"), True, nc.gpsimd, "pool")
        self.sp = _S(mk("s_sp"), False, nc.sync, "sp")
        self.engs = [self.pe, self.act, self.dve, self.pool, self.sp]

    def _reg(self, k):
        r = self.regs.get(k)
        if r is None:
            r = self.regs[k] = _Reg()
        return r

    def _collect(self, E, r, w):
        need = {}
        for k in r:
            rg = self._reg(k)
            for S_, v in rg.writers.items():
                if need.get(S_, 0) < v:
                    need[S_] = v
            if isinstance(k, tuple) and k[0] == "ps":
                for S_, v in rg.readers.items():
                    if S_ is not E and need.get(S_, 0) < v:
                        need[S_] = v
        for k in w:
            rg = self._reg(k)
            for S_, v in rg.writers.items():
                if need.get(S_, 0) < v:
                    need[S_] = v
            for S_, v in rg.readers.items():
                if need.get(S_, 0) < v:
                    need[S_] = v
        for S_, v in need.items():
            if S_ is E and not E.selfsync:
                continue
            if E.seen.get(S_, 0) >= v:
                continue
            E.seen[S_] = v
            E.h.wait_ge(S_.sem, v)

    def _update(self, S_, val, r, w):
        for k in r:
            self._reg(k).readers[S_] = val
        for k in w:
            rg = self._reg(k)
            rg.writers = {S_: val}
            rg.readers = {}

    def op(self, E, fn, r=(), w=()):
        self._collect(E, r, w)
        ins = fn(E.h)
        E.val += 1
        ins.then_inc(E.sem, 1)
        self._update(E, E.val, r, w)

    def PE(self, fn, r=(), w=()):
        self.op(self.pe, fn, r, w)

    def ACT(self, fn, r=(), w=()):
        self.op(self.act, fn, r, w)

    def DVE(self, fn, r=(), w=()):
        self.op(self.dve, fn, r, w)

    def POOL(self, fn, r=(), w=()):
        self.op(self.pool, fn, r, w)

    def dma(self, q, out, in_, r=(), w=(), semkey=None, **kw):
        E = self.pool if q == "pool" else self.sp
        if semkey is None:
            semkey = ("d", w[0] if w else r[0])
        ds = self.dsems.get(semkey)
        if ds is None:
            ds = self.dsems[semkey] = _S(self.es.enter_context(self.nc.semaphore("sd%d" % len(self.dsems))), name=str(semkey))
        self._collect(E, r, w)
        E.h.dma_start(out=out, in_=in_, **kw).then_inc(ds.sem, 16)
        ds.val += 16
        self._update(ds, ds.val, r, w)
        self.groups.setdefault(semkey, []).extend(list(w))
        return ds

    def finalize(self, semkey):
        ds = self.dsems[semkey]
        for k in self.groups.get(semkey, []):
            rg = self._reg(k)
            if ds in rg.writers:
                rg.writers[ds] = ds.val

    def barrier(self):
        allS = self.engs[:4] + list(self.dsems.values())
        for E in self.engs:
            for S_ in allS:
                if S_ is E or S_.val == 0:
                    continue
                if E.seen.get(S_, 0) >= S_.val:
                    continue
                E.seen[S_] = S_.val
                E.h.wait_ge(S_.sem, S_.val)

    def final_wait(self):
        E = self.sp
        for S_ in self.engs[:4] + list(self.dsems.values()):
            if S_.val and E.seen.get(S_, 0) < S_.val:
                E.h.wait_ge(S_.sem, S_.val)


def build_nc():
    nc = bass.Bass("TRN2", target_bir_lowering=False)
    dt = lambda n, s, d=F32, k="ExternalInput": nc.dram_tensor(n, list(s), d, kind=k).ap()
    xs = dt("xs", [S, D])
    posr = dt("posr", [64, S], I32)
    valid = dt("valid", [128, NT])
    cT = dt("cT", [128, KD])
    w_ada = dt("w_ada", [D, 6 * D])
    b_ada = dt("b_ada", [96, 128])
    w_ada_f = dt("w_ada_final", [D, 2 * D])
    b_ada_f = dt("b_ada_final", [32, 128])
    vecs = dt("vecs", [54, 128])
    w_in = dt("w_in", [D, 8016])
    w_q_b = dt("mla_w_q_b", [512, 1536])
    w_kv_b = dt("mla_w_kv_b", [256, 2048])
    wgk1 = dt("wgk1", [17, 512])
    gla_nw = dt("gla_nw", [128, 256])
    w_o_mla = dt("w_o_mla", [1024, D])
    w_o_gla = dt("w_o_gla", [1024, D])
    w_out = dt("w_out", [D, D])
    w_router = dt("w_router", [D, NE])
    b_router = dt("b_router", [128, NE])
    if STOP > 4:
        w1 = dt("w1", [NE, D, 2 * D])
        b1 = dt("b1", [NE, 2 * D])
        w2 = dt("w2", [NE, D, D])
        b2 = dt("b2", [NE, D])
    c_ident = dt("c_ident", [128, 128])
    c_tri = dt("c_tri", [128, 256])
    c_mask = dt("c_mask", [128, 4 * 512])
    c_rope = dt("c_rope", [64, 2])
    out = dt("out", [1024, D], F32, "ExternalOutput")
    x1d = dt("x1d", [1024, D], F32, "Internal")
    glao_d = dt("glao_d", [128, 8192], BF16, "Internal")
    mlao_d = dt("mlao_d", [128, 8192], BF16, "Internal")
    dbg = {}
    if DEBUG:
        for nm, shp in [("d_mod", [128, 128]), ("d_hT", [128, 16 * 128]), ("d_ckvn", [128, 2 * S]), ("d_krot", [64, S]),
                        ("d_S", [128, 1024]), ("d_on", [128, 8 * 1024]), ("d_glao", [128, 8 * 1024]),
                        ("d_qn", [128, 8 * 1024]), ("d_qrot", [64, 8 * 1024]), ("d_mlao", [128, 8 * 1024]),
                        ("d_merged", [128, 16 * 1024]), ("d_x1", [128, 8 * 2048]), ("d_G", [128, 8 * 32]),
                        ("d_acc", [128, 8 * 2048])]:
            dbg[nm] = dt(nm, shp, F32, "ExternalOutput")

    with ExitStack() as es:
        P = Prog(nc, es)
        sb = lambda n, s, d=F32, st=es: st.enter_context(nc.sbuf_tensor(n, list(s), d))
        pst = lambda n, s, d=F32, st=es: st.enter_context(nc.psum_tensor(n, list(s), d))
        block = es.enter_context(nc.Block())

        def body(_):
            ident = sb("ident", [128, 128])
            identb = sb("identb", [128, 128], BF16)
            ones_f = sb("ones_f", [128, 128])
            ones_b = sb("ones_b", [128, 128], BF16)
            tri = sb("tri", [128, 256])
            cmask = sb("cmask", [128, 2048], BF16)
            crope = sb("crope", [64, 2])
            valid_sb = sb("valid_sb", [128, NT])
            kbias = sb("kbias", [128, NT])
            modT = sb("modT", [128, 128])
            vT = sb("vT", [128, 64])
            AB = sb("AB", [128, 6 * 16])
            wgk1_sb = sb("wgk1_sb", [17, 512])
            glanw = sb("glanw", [128, 256])
            brout = sb("brout", [128, NE])
            G = sb("G", [128, 8, NE])
            eps_t = sb("eps_t", [128, 1])
            one_t = sb("one_t", [128, 1])
            PS = [pst("ps%d" % i, [128, 512]) for i in range(8)]
            pk = lambda i: ("ps", i)

            def dump(name, ap, key, cols):
                if not DEBUG or name not in dbg:
                    return
                for c0 in range(0, cols, 2048):
                    c1 = min(cols, c0 + 2048)
                    P.dma("pool", dbg[name][0:ap.shape[0], c0:c1], ap[:, c0:c1], r=[key], semkey=("dbg", name))

            ck = "const"
            P.dma("sp", ident[:], c_ident[:, :], w=["ident"], semkey=ck)
            P.dma("sp", tri[:], c_tri[:, :], w=["tri"], semkey=ck)
            P.dma("sp", crope[:], c_rope[:, :], w=["crope"], semkey=ck)
            P.dma("sp", valid_sb[:], valid[:, :], w=["valid"], semkey=ck)
            P.dma("sp", wgk1_sb[:], wgk1[:, :], w=["wgk1"], semkey=ck)
            P.dma("sp", glanw[:], gla_nw[:, :], w=["glanw"], semkey=ck)
            P.dma("sp", brout[:], b_router[:, :], w=["brout"], semkey=ck)
            P.dma("pool", cmask[:], c_mask[:, :], w=["cmask"], semkey="constp")
            P.finalize(ck)
            P.finalize("constp")
            P.DVE(lambda e: e.memset(ones_f[:], 1.0), w=["ones_f"])
            P.DVE(lambda e: e.memset(ones_b[:], 1.0), w=["ones_b"])
            P.DVE(lambda e: e.memset(eps_t[:], EPS), w=["eps_t"])
            P.DVE(lambda e: e.memset(one_t[:], 1.0), w=["one_t"])
            P.DVE(lambda e: e.tensor_copy(out=identb[:], in_=ident[:]), r=["ident"], w=["identb"])
            P.DVE(lambda e: e.tensor_scalar(out=kbias[:], in0=valid_sb[:], scalar1=-1.0, scalar2=30000.0,
                                            op0=ALU.add, op1=ALU.mult), r=["valid"], w=["kbias"])

            with ExitStack() as ph:
                cin = sb("cin", [128, KD], F32, ph)
                cond = sb("cond", [128, KD], BF16, ph)
                vrow = sb("vrow", [128, 128], F32, ph)
                vrow2 = sb("vrow2", [54, 128], F32, ph)
                wbuf = [sb("wada%d" % i, [128, KD, 512], BF16, ph) for i in range(2)]
                P.dma("sp", cin[:], cT[:, :], w=["cin"])
                P.dma("sp", vrow[0:96, :], b_ada[:, :], w=["vrow"], semkey="vr")
                P.dma("sp", vrow[96:128, :], b_ada_f[:, :], w=["vrow"], semkey="vr")
                P.dma("sp", vrow2[:], vecs[:, :], w=["vrow2"], semkey="vr")
                P.finalize("vr")
                P.ACT(lambda e: e.activation(out=cond[:], in_=cin[:], func=AF.Silu), r=["cin"], w=["cond"])
                nblk = 32
                for bi in range(nblk):
                    wb = wbuf[bi % 2]
                    src = (w_ada[:, bi * 512:(bi + 1) * 512] if bi < 24 else w_ada_f[:, (bi - 24) * 512:(bi - 23) * 512])
                    P.dma("pool", wb[:], src.rearrange("(k p) n -> p k n", p=128), w=[("wada", bi % 2)])
                    for jj in range(4):
                        j = bi * 4 + jj
                        for k in range(KD):
                            P.PE(lambda e, k=k, jj=jj, j=j, wb=wb: e.matmul(
                                PS[0][:, j:j + 1], wb[:, k, jj * 128:(jj + 1) * 128], cond[:, k:k + 1],
                                start=(k == 0), stop=(k == KD - 1)),
                                r=[("wada", bi % 2), "cond"], w=[pk(0)])
                P.PE(lambda e: e.transpose(PS[1][:, 0:128], vrow[:], ident[:]), r=["vrow", "ident"], w=[pk(1)])
                P.PE(lambda e: e.transpose(PS[1][:, 128:128 + 54], vrow2[:], ident[0:54, 0:54]), r=["vrow2", "ident"], w=[pk(1)])
                P.ACT(lambda e: e.activation(out=vT[:, 0:54], in_=PS[1][:, 128:128 + 54], func=AF.Copy), r=[pk(1)], w=["vT"])
                P.ACT(lambda e: e.activation(func=AF.Copy, out=modT[:], in_=PS[1][:, 0:128]), r=[pk(1)], w=["modT"])
                P.DVE(lambda e: e.tensor_tensor(out=modT[:], in0=PS[0][:, 0:128], in1=modT[:], op=ALU.add),
                      r=[pk(0), "modT"], w=["modT"])
                for idx, (nw0, sc0, sh0) in enumerate([(0, 16, 0), (16, 64, 48), (32, 112, 96)]):
                    a_ap = AB[:, (2 * idx) * 16:(2 * idx + 1) * 16]
                    b_ap = AB[:, (2 * idx + 1) * 16:(2 * idx + 2) * 16]
                    P.DVE(lambda e, a_ap=a_ap, sc0=sc0, nw0=nw0: e.scalar_tensor_tensor(
                        out=a_ap, in0=modT[:, sc0:sc0 + 16], scalar=1.0, in1=vT[:, nw0:nw0 + 16],
                        op0=ALU.add, op1=ALU.mult), r=["modT", "vT"], w=["AB"])
                    P.DVE(lambda e, b_ap=b_ap, sh0=sh0: e.tensor_copy(out=b_ap, in_=modT[:, sh0:sh0 + 16]),
                          r=["modT"], w=["AB"])
                dump("d_mod", modT[:], "modT", 128)
                P.barrier()

            if STOP <= 0:
                return
            A_m, B_m = AB[:, 0:16], AB[:, 16:32]
            A_f, B_f = AB[:, 32:48], AB[:, 48:64]
            A_o, B_o = AB[:, 64:80], AB[:, 80:96]
            qnw = vT[:, 48:52]
            kvnw = vT[:, 52:54]

            def make_hT(xt, xk, dst_fn, dkeys, Acol, Bcol, stat, f32dst_fn=None, f32keys=None):
                junk = stat["junk"]
                P.ACT(lambda e: e.activation(out=junk[:], in_=xt, func=AF.Square, accum_out=stat["t"][:, 0:1]),
                      r=[xk], w=["junk", "stat"])
                P.ACT(lambda e: e.activation(out=stat["t"][:, 1:2], in_=stat["t"][:, 0:1], func=AF.Sqrt,
                                             bias=eps_t[:], scale=1.0 / D), r=["stat", "eps_t"], w=["stat"])
                P.DVE(lambda e: e.reciprocal(out=stat["t"][:, 2:3], in_=stat["t"][:, 1:2]), r=["stat"], w=["stat"])
                P.DVE(lambda e: e.tensor_scalar(out=xt, in0=xt, scalar1=stat["t"][:, 2:3], scalar2=None, op0=ALU.mult),
                      r=[xk, "stat"], w=[xk])
                for g4 in range(4):
                    bank = 4 + (g4 % 2)
                    for i in range(4):
                        c = g4 * 4 + i
                        P.PE(lambda e, c=c, i=i, bank=bank: e.transpose(PS[bank][:, i * 128:(i + 1) * 128],
                                                                          xt[:, c * 128:(c + 1) * 128], ident[:]),
                             r=[xk, "ident"], w=[pk(bank)])
                    for i in range(4):
                        c = g4 * 4 + i
                        src = PS[bank][:, i * 128:(i + 1) * 128]
                        if i % 2 == 0:
                            P.ACT(lambda e, c=c, src=src: e.activation(out=dst_fn(c), in_=src, func=AF.Identity,
                                                                       bias=Bcol[:, c:c + 1], scale=Acol[:, c:c + 1]),
                                  r=[pk(bank), "AB"], w=[dkeys[c]])
                        else:
                            P.DVE(lambda e, c=c, src=src: e.tensor_scalar(out=dst_fn(c), in0=src, scalar1=Acol[:, c:c + 1],
                                                                          scalar2=Bcol[:, c:c + 1], op0=ALU.mult, op1=ALU.add),
                                  r=[pk(bank), "AB"], w=[dkeys[c]])
                        if f32dst_fn is not None:
                            P.DVE(lambda e, c=c, src=src: e.tensor_scalar(out=f32dst_fn(c), in0=src, scalar1=Acol[:, c:c + 1],
                                                                          scalar2=Bcol[:, c:c + 1], op0=ALU.mult, op1=ALU.add),
                                  r=[pk(bank), "AB"], w=[f32keys[c]])

            mix = ExitStack()
            ckvn = sb("ckvn", [128, 2, S], BF16, mix)
            krot = sb("krot", [128, S], BF16, mix)
            o_n = sb("o_n", [128, 8, 1024], BF16, mix)
            cs_own = sb("cs_own", [64, 2, 1024], F32, mix)

            with ExitStack() as ph:
                WA = sb("WA", [128, KD, WA_N], BF16, ph)
                xt_b = [sb("xt%d" % i, [128, D], F32, ph) for i in range(2)]
                hT_b = [sb("hT%d" % i, [128, KD, 128], BF16, ph) for i in range(2)]
                junk = sb("junk", [128, D], BF16, ph)
                statt = sb("statt", [128, 4], F32, ph)
                stat = {"junk": junk, "t": statt}
                posi = sb("posi", [64, 128], I32, ph)
                ang = sb("ang", [64, 6, 128], F32, ph)
                ckvf = sb("ckvf", [128, 2, 128], F32, ph)
                sq = sb("sq", [128, 2, 128], BF16, ph)
                rinv = sb("rinv", [128, 2, 128], F32, ph)
                rtmp = sb("rtmp", [64, 2, 128], F32, ph)
                glrT1 = sb("glrT1", [17, 128], F32, ph)
                lsp = sb("lsp", [128, 2, 512], F32, ph)
                ek = sb("ek", [128, 512], F32, ph)
                kp_tok = sb("kp_tok", [128, 512], BF16, ph)
                v_tok = sb("v_tok", [128, 1024], BF16, ph)
                ebT = sb("ebT", [128, 2, 512], F32, ph)
                qpT = sb("qpT", [128, 512], BF16, ph)
                kppT = sb("kppT", [128, 512], BF16, ph)
                AT = sb("AT", [128, 512], BF16, ph)
                Sst = sb("Sst", [128, 1024], F32, ph)
                Sbf = sb("Sbf", [128, 1024], BF16, ph)
                ostat = sb("ostat", [128, 16], F32, ph)
                otmp = sb("otmp", [128, 1024], F32, ph)
                for (a, b_, c0) in [(WA_CKV, 320, C_CKV), (WA_GK, 512, C_GK), (WA_GV, 512, C_GV), (WA_GV + 512, 512, C_GV + 512),
                                    (WA_GLR, 16, C_GLR), (WA_GQ, 512, C_GQ)]:
                    P.dma("pool", WA[:, :, a:a + b_], w_in[:, c0:c0 + b_].rearrange("(k p) n -> p k n", p=128),
                          w=["WA"], semkey="WA")
                P.finalize("WA")
                P.DVE(lambda e: e.memset(glrT1[:], 1.0), w=["glrT1"])
                P.DVE(lambda e: e.memset(krot[64:128, :], 0.0), w=["krotpad"])
                P.DVE(lambda e: e.memset(Sst[:], 0.0), w=["Sst"])
                P.DVE(lambda e: e.memset(Sbf[:], 0.0), w=["Sbf"])
                hkeys = lambda bi: [("hT", bi, c) for c in range(KD)]
                def prep(t):
                    bi = t % 2
                    xt, hT = xt_b[bi], hT_b[bi]
                    xk = ("xt", bi)
                    P.dma("sp", xt[:], xs[t * 128:(t + 1) * 128, :], w=[xk])
                    make_hT(xt[:], xk, lambda c, hT=hT: hT[:, c, :], hkeys(bi), A_m, B_m, stat)

                prep(0)
                for t in range(NT):
                    own = t >= OWN0
                    bi = t % 2
                    xt, hT = xt_b[bi], hT_b[bi]
                    xk = ("xt", bi)
                    if t + 1 < NT:
                        prep(t + 1)
                    HK = hkeys(bi)
                    P.dma("sp", posi[:], posr[:, t * 128:(t + 1) * 128], w=["posi"])
                    a0, a1, a2, a3, a4, a5 = [ang[:, i, :] for i in range(6)]
                    P.DVE(lambda e: e.tensor_copy(out=a0, in_=posi[:]), r=["posi"], w=["ang0"])
                    P.DVE(lambda e: e.tensor_scalar(out=a0, in0=a0, scalar1=crope[:, 0:1], scalar2=None, op0=ALU.mult),
                          r=["ang0", "crope"], w=["ang0"])
                    MAGIC = 12582912.0
                    C1, C2 = 6.28125, 0.0019353071795864769
                    for which, off, dstc in [("sin", 0.0, a4), ("cos", 0.25, a5)]:
                        P.DVE(lambda e, off=off: e.tensor_scalar(out=a1, in0=a0, scalar1=1.0 / (2 * np.pi), scalar2=off,
                                                                 op0=ALU.mult, op1=ALU.add), r=["ang0"], w=["ang1"])
                        P.DVE(lambda e: e.tensor_scalar(out=a1, in0=a1, scalar1=MAGIC, scalar2=None, op0=ALU.add),
                              r=["ang1"], w=["ang1"])
                        P.DVE(lambda e: e.tensor_scalar(out=a1, in0=a1, scalar1=-MAGIC, scalar2=None, op0=ALU.add),
                              r=["ang1"], w=["ang1"])
                        P.DVE(lambda e: e.scalar_tensor_tensor(out=a2, in0=a1, scalar=-C1, in1=a0, op0=ALU.mult, op1=ALU.add),
                              r=["ang1", "ang0"], w=["ang2"])
                        P.DVE(lambda e: e.scalar_tensor_tensor(out=a2, in0=a1, scalar=-C2, in1=a2, op0=ALU.mult, op1=ALU.add),
                              r=["ang1", "ang2"], w=["ang2"])
                        P.DVE(lambda e, off=off: e.tensor_scalar(out=a2, in0=a2, scalar1=off * 2 * np.pi, scalar2=3.1415925,
                                                                 op0=ALU.add, op1=ALU.min), r=["ang2"], w=["ang2"])
                        P.DVE(lambda e: e.tensor_scalar(out=a2, in0=a2, scalar1=-3.1415925, scalar2=None, op0=ALU.max),
                              r=["ang2"], w=["ang2"])
                        P.ACT(lambda e, dstc=dstc: e.activation(out=dstc, in_=a2, func=AF.Sin), r=["ang2"], w=["angcs"])
                    P.DVE(lambda e: e.tensor_scalar(out=a4, in0=a4, scalar1=crope[:, 1:2], scalar2=None, op0=ALU.mult),
                          r=["angcs", "crope"], w=["angcs"])
                    if own:
                        oo = (t - OWN0) * 128
                        P.DVE(lambda e, oo=oo: e.tensor_copy(out=cs_own[:, 0, oo:oo + 128], in_=a5), r=["angcs"], w=["cs_own"])
                        P.DVE(lambda e, oo=oo: e.tensor_copy(out=cs_own[:, 1, oo:oo + 128], in_=a4), r=["angcs"], w=["cs_own"])
                    for c in range(2):
                        for k in range(KD):
                            P.PE(lambda e, c=c, k=k: e.matmul(PS[0][:, c * 128:(c + 1) * 128], WA[:, k, WA_CKV + c * 128:WA_CKV + (c + 1) * 128],
                                                               hT[:, k, :], start=(k == 0), stop=(k == KD - 1)),
                                 r=["WA"] + HK, w=[pk(0)])
                    for v_ in range(2):
                        for half in range(2):
                            par = half if v_ == 0 else 1 - half
                            for k in range(KD):
                                lhs = WA[:, k, WA_KPE + par:WA_KPE + 64:2]
                                P.PE(lambda e, v_=v_, k=k, lhs=lhs, half=half: e.matmul(
                                    PS[1][half * 32:(half + 1) * 32, v_ * 128:(v_ + 1) * 128], lhs, hT[:, k, :],
                                    start=(k == 0), stop=(k == KD - 1)), r=["WA"] + HK, w=[pk(1)])
                    P.ACT(lambda e: e.activation(func=AF.Copy, out=ckvf[:].rearrange("p c t -> p (c t)"), in_=PS[0][:, 0:256]), r=[pk(0)], w=["ckvf"])
                    P.ACT(lambda e: e.activation(out=sq[:].rearrange("p c t -> p (c t)"), in_=PS[0][:, 0:256], func=AF.Square),
                          r=[pk(0)], w=["sq"])
                    for c in range(2):
                        P.PE(lambda e, c=c: e.matmul(PS[2][:, 0:128], ones_b[:], sq[:, c, :], start=(c == 0), stop=(c == 1)),
                             r=["ones_b", "sq"], w=[pk(2)])
                    P.ACT(lambda e: e.activation(out=rinv[:, 0, :], in_=PS[2][:, 0:128], func=AF.Sqrt, bias=eps_t[:], scale=1.0 / 256),
                          r=[pk(2), "eps_t"], w=["rinv"])
                    P.DVE(lambda e: e.reciprocal(out=rinv[:, 1, :], in_=rinv[:, 0, :]), r=["rinv"], w=["rinv"])
                    for c in range(2):
                        P.DVE(lambda e, c=c: e.scalar_tensor_tensor(out=ckvn[:, c, t * 128:(t + 1) * 128], in0=ckvf[:, c, :],
                                                                   scalar=kvnw[:, c:c + 1], in1=rinv[:, 1, :], op0=ALU.mult, op1=ALU.mult),
                              r=["ckvf", "rinv", "vT"], w=[("ckvn", t)])
                    P.DVE(lambda e: e.tensor_tensor(out=rtmp[:, 0, :], in0=PS[1][0:64, 0:128], in1=a5, op=ALU.mult),
                          r=[pk(1), "angcs"], w=["rtmp"])
                    P.DVE(lambda e: e.tensor_tensor(out=rtmp[:, 1, :], in0=PS[1][0:64, 128:256], in1=a4, op=ALU.mult),
                          r=[pk(1), "angcs"], w=["rtmp"])
                    P.DVE(lambda e: e.tensor_tensor(out=krot[0:64, t * 128:(t + 1) * 128], in0=rtmp[:, 0, :], in1=rtmp[:, 1, :], op=ALU.add),
                          r=["rtmp"], w=[("krot", t)])
                    for k in range(KD):
                        P.PE(lambda e, k=k: e.matmul(PS[2][0:16, 128:256], WA[:, k, WA_GLR:WA_GLR + 16], hT[:, k, :],
                                                     start=(k == 0), stop=(k == KD - 1)), r=["WA"] + HK, w=[pk(2)])
                    P.ACT(lambda e: e.activation(out=glrT1[0:16, :], in_=PS[2][0:16, 128:256], func=AF.Copy), r=[pk(2)], w=["glrT1"])
                    P.PE(lambda e: e.matmul(PS[3][:, :], glrT1[:], wgk1_sb[:], start=True, stop=True), r=["glrT1", "wgk1"], w=[pk(3)])
                    P.ACT(lambda e: e.activation(out=lsp[:, 0, :], in_=PS[3][:, :], func=AF.Exp, scale=-1.0), r=[pk(3)], w=["lsp0"])
                    P.ACT(lambda e: e.activation(out=lsp[:, 1, :], in_=lsp[:, 0, :], func=AF.Ln, bias=one_t[:], scale=1.0),
                          r=["lsp0", "one_t"], w=["lsp"])
                    L = lsp[:, 1, :]
                    P.PE(lambda e: e.matmul(PS[3][:, :], tri[:, 128:256], L, start=True, stop=True), r=["tri", "lsp"], w=[pk(3)])
                    for hd in range(4):
                        P.PE(lambda e, hd=hd: e.matmul(PS[2][:, hd * 128:(hd + 1) * 128], L[:, hd * 128:(hd + 1) * 128], tri[:, 0:128],
                                                       start=True, stop=True), r=["tri", "lsp"], w=[pk(2)])
                    P.ACT(lambda e: e.activation(out=ek[:], in_=PS[3][:, :], func=AF.Exp), r=[pk(3)], w=["ek"])
                    P.ACT(lambda e: e.activation(out=ebT[:, 0, :], in_=PS[2][:, :], func=AF.Exp), r=[pk(2)], w=["ebT"])
                    if own:
                        P.ACT(lambda e: e.activation(out=ebT[:, 1, :], in_=PS[2][:, :], func=AF.Exp, scale=-1.0), r=[pk(2)], w=["ebTn"])
                    for k in range(KD):
                        P.PE(lambda e, k=k: e.matmul(PS[3][:, :], hT[:, k, :], WA[:, k, WA_GK:WA_GK + 512], start=(k == 0), stop=(k == KD - 1)),
                             r=["WA"] + HK, w=[pk(3)])
                    P.DVE(lambda e: e.tensor_tensor(out=kp_tok[:], in0=PS[3][:, :], in1=ek[:], op=ALU.mult), r=[pk(3), "ek"], w=["kp_tok"])
                    for hh in range(2):
                        for k in range(KD):
                            P.PE(lambda e, k=k, hh=hh: e.matmul(PS[6 + hh][:, :], hT[:, k, :], WA[:, k, WA_GV + hh * 512:WA_GV + (hh + 1) * 512],
                                                                 start=(k == 0), stop=(k == KD - 1)), r=["WA"] + HK, w=[pk(6 + hh)])
                        P.ACT(lambda e, hh=hh: e.activation(out=v_tok[:, hh * 512:(hh + 1) * 512], in_=PS[6 + hh][:, :], func=AF.Copy,
                                                           scale=valid_sb[:, t:t + 1]), r=[pk(6 + hh), "valid"], w=["v_tok"])
                    if own:
                        for hd in range(4):
                            for k in range(KD):
                                P.PE(lambda e, k=k, hd=hd: e.matmul(PS[0][:, hd * 128:(hd + 1) * 128], WA[:, k, WA_GQ + hd * 128:WA_GQ + (hd + 1) * 128],
                                                                     hT[:, k, :], start=(k == 0), stop=(k == KD - 1)), r=["WA"] + HK, w=[pk(0)])
                        for hd in range(4):
                            for k in range(KD):
                                P.PE(lambda e, k=k, hd=hd: e.matmul(PS[1][:, hd * 128:(hd + 1) * 128], WA[:, k, WA_GK + hd * 128:WA_GK + (hd + 1) * 128],
                                                                     hT[:, k, :], start=(k == 0), stop=(k == KD - 1)), r=["WA"] + HK, w=[pk(1)])
                        P.DVE(lambda e: e.scalar_tensor_tensor(out=qpT[:], in0=PS[0][:, :], scalar=128.0 ** -0.5, in1=ebT[:, 0, :],
                                                               op0=ALU.mult, op1=ALU.mult), r=[pk(0), "ebT"], w=["qpT"])
                        P.DVE(lambda e: e.tensor_tensor(out=kppT[:], in0=PS[1][:, :], in1=ebT[:, 1, :], op=ALU.mult),
                              r=[pk(1), "ebTn"], w=["kppT"])
                        for hd in range(4):
                            P.PE(lambda e, hd=hd: e.matmul(PS[0][:, hd * 128:(hd + 1) * 128], kppT[:, hd * 128:(hd + 1) * 128],
                                                           qpT[:, hd * 128:(hd + 1) * 128], start=True, stop=True), r=["kppT", "qpT"], w=[pk(0)])
                        P.DVE(lambda e: e.tensor_tensor(out=AT[:].rearrange("p (h t) -> p h t", h=4), in0=PS[0][:, :].rearrange("p (h t) -> p h t", h=4),
                                                        in1=cmask[:, 0:128].unsqueeze(1).to_broadcast([128, 4, 128]), op=ALU.mult),
                              r=[pk(0), "cmask"], w=["AT"])
                        for hd in range(4):
                            bank = hd // 2
                            oc = (hd % 2) * 256
                            P.PE(lambda e, hd=hd, bank=bank, oc=oc: e.matmul(PS[bank][:, oc:oc + 256], qpT[:, hd * 128:(hd + 1) * 128],
                                                                           Sbf[:, hd * 256:(hd + 1) * 256], start=True, stop=False),
                                 r=["qpT", "Sbf"], w=[pk(bank)])
                            P.PE(lambda e, hd=hd, bank=bank, oc=oc: e.matmul(PS[bank][:, oc:oc + 256], AT[:, hd * 128:(hd + 1) * 128],
                                                                           v_tok[:, hd * 256:(hd + 1) * 256], start=False, stop=True),
                                 r=["AT", "v_tok"], w=[pk(bank)])
                        tt = t - OWN0
                        for hd in range(4):
                            bank = hd // 2
                            oc = (hd % 2) * 256
                            P.ACT(lambda e, hd=hd, bank=bank, oc=oc: e.activation(out=otmp[:, hd * 256:(hd + 1) * 256], in_=PS[bank][:, oc:oc + 256],
                                                                                func=AF.Square, accum_out=ostat[:, hd:hd + 1]),
                                  r=[pk(bank)], w=["otmp", "ostat"])
                        P.ACT(lambda e: e.activation(out=ostat[:, 4:8], in_=ostat[:, 0:4], func=AF.Sqrt, bias=eps_t[:], scale=1.0 / 256),
                              r=["ostat", "eps_t"], w=["ostat"])
                        P.DVE(lambda e: e.reciprocal(out=ostat[:, 8:12], in_=ostat[:, 4:8]), r=["ostat"], w=["ostat"])
                        for hd in range(4):
                            bank = hd // 2
                            oc = (hd % 2) * 256
                            P.DVE(lambda e, hd=hd, bank=bank, oc=oc: e.scalar_tensor_tensor(
                                out=o_n[:, tt, hd * 256:(hd + 1) * 256], in0=PS[bank][:, oc:oc + 256], scalar=ostat[:, 8 + hd:9 + hd],
                                in1=glanw[:], op0=ALU.mult, op1=ALU.mult), r=[pk(bank), "ostat", "glanw"], w=[("o_n", tt)])
                    for hd in range(4):
                        bank = 6 + hd // 2
                        oc = (hd % 2) * 256
                        P.PE(lambda e, hd=hd, bank=bank, oc=oc: e.matmul(PS[bank][:, oc:oc + 256], kp_tok[:, hd * 128:(hd + 1) * 128],
                                                                       v_tok[:, hd * 256:(hd + 1) * 256], start=True, stop=True),
                             r=["kp_tok", "v_tok"], w=[pk(bank)])
                    for hd in range(4):
                        bank = 6 + hd // 2
                        oc = (hd % 2) * 256
                        P.DVE(lambda e, hd=hd, bank=bank, oc=oc: e.scalar_tensor_tensor(
                            out=Sst[:, hd * 256:(hd + 1) * 256], in0=Sst[:, hd * 256:(hd + 1) * 256],
                            scalar=ebT[:, 0, hd * 128 + 127:hd * 128 + 128], in1=PS[bank][:, oc:oc + 256], op0=ALU.mult, op1=ALU.add),
                            r=[pk(bank), "ebT", "Sst"], w=["Sst"])
                    if t >= OWN0 - 1:
                        P.ACT(lambda e: e.activation(func=AF.Copy, out=Sbf[:], in_=Sst[:]), r=["Sst"], w=["Sbf"])
                    if DEBUG and t == OWN0 - 1:
                        dump("d_S", Sst[:], "Sst", 1024)
                dump("d_ckvn", ckvn[:].rearrange("p c s -> p (c s)"), ("ckvn", 0), 2 * S)
                dump("d_krot", krot[0:64, :], ("krot", 0), S)
                dump("d_on", o_n[:].rearrange("p a b -> p (a b)"), ("o_n", 0), 8192)
                P.barrier()

            if STOP <= 1:
                mix.close()
                return
            mixB = ExitStack()
            glaoT = sb("glaoT", [128, 8, 1024], BF16, mixB)
            qnT = sb("qnT", [128, 8, 1024], BF16, mixB)
            qrotT = sb("qrotT", [128, 8, 1024], BF16, mixB)
            SC = 192.0 ** -0.5
            P.DVE(lambda e: e.memset(qrotT[64:128, :, :], 0.0), w=["qrotpad"])
            with ExitStack() as ph:
                WB = sb("WB", [128, KD, 1536], BF16, ph)
                wqb = sb("wqb", [128, 4, 1536], BF16, ph)
                xt = sb("xt2", [128, D], F32, ph)
                hT = sb("hT2", [128, KD, 128], BF16, ph)
                junk = sb("junk2", [128, D], BF16, ph)
                statt = sb("statt2", [128, 4], F32, ph)
                stat = {"junk": junk, "t": statt}
                sg = sb("sg", [128, 1024], F32, ph)
                gtok = sb("gtok", [128, 1024], F32, ph)
                qlf = sb("qlf", [128, 512], F32, ph)
                sq2 = sb("sq2", [128, 512], BF16, ph)
                rv = sb("rv", [128, 2, 128], F32, ph)
                qlatn = sb("qlatn", [128, 4, 128], BF16, ph)
                rt = sb("rt", [64, 2, 512], F32, ph)
                for (a, n, c0) in [(0, 512, C_GOUT), (512, 512, C_GOUT + 512), (1024, 512, C_QLAT)]:
                    P.dma("pool", WB[:, :, a:a + n], w_in[:, c0:c0 + n].rearrange("(k p) n -> p k n", p=128), w=["WB"], semkey="WB")
                P.finalize("WB")
                P.dma("pool", wqb[:], w_q_b[:, :].rearrange("(k p) n -> p k n", p=128), w=["wqb"])
                HK2 = [("hT2", c) for c in range(KD)]
                for tt in range(8):
                    tsl = slice(tt * 128, (tt + 1) * 128)
                    P.dma("sp", xt[:], xs[(OWN0 + tt) * 128:(OWN0 + tt + 1) * 128, :], w=["xt2"])
                    make_hT(xt[:], "xt2", lambda c: hT[:, c, :], HK2, A_m, B_m, stat)
                    for hh in range(2):
                        for k in range(KD):
                            P.PE(lambda e, k=k, hh=hh: e.matmul(PS[6 + hh][:, :], hT[:, k, :], WB[:, k, hh * 512:(hh + 1) * 512],
                                                                 start=(k == 0), stop=(k == KD - 1)), r=["WB", HK2[k]], w=[pk(6 + hh)])
                        P.ACT(lambda e, hh=hh: e.activation(out=sg[:, hh * 512:(hh + 1) * 512], in_=PS[6 + hh][:, :], func=AF.Silu),
                              r=[pk(6 + hh)], w=["sg"])
                    P.DVE(lambda e, tt=tt: e.tensor_tensor(out=gtok[:], in0=sg[:], in1=o_n[:, tt, :], op=ALU.mult),
                          r=["sg", ("o_n", tt)], w=["gtok"])
                    for g2 in range(2):
                        for i in range(4):
                            c = g2 * 4 + i
                            P.PE(lambda e, c=c, i=i, g2=g2: e.transpose(PS[6 + g2][:, i * 128:(i + 1) * 128], gtok[:, c * 128:(c + 1) * 128], ident[:]),
                                 r=["gtok", "ident"], w=[pk(6 + g2)])
                        P.ACT(lambda e, g2=g2, tsl=tsl: e.activation(out=glaoT[:, g2 * 4:(g2 + 1) * 4, tsl],
                                                                    in_=PS[6 + g2][:, :].rearrange("p (c t) -> p c t", c=4), func=AF.Copy),
                              r=[pk(6 + g2)], w=[("glaoT", tt)])
                    for c in range(4):
                        for k in range(KD):
                            P.PE(lambda e, k=k, c=c: e.matmul(PS[0][:, c * 128:(c + 1) * 128], WB[:, k, 1024 + c * 128:1024 + (c + 1) * 128], hT[:, k, :],
                                                               start=(k == 0), stop=(k == KD - 1)), r=["WB", HK2[k]], w=[pk(0)])
                    P.ACT(lambda e: e.activation(out=qlf[:], in_=PS[0][:, :], func=AF.Copy), r=[pk(0)], w=["qlf"])
                    P.ACT(lambda e: e.activation(out=sq2[:], in_=PS[0][:, :], func=AF.Square), r=[pk(0)], w=["sq2"])
                    for c in range(4):
                        P.PE(lambda e, c=c: e.matmul(PS[1][:, 0:128], ones_b[:], sq2[:, c * 128:(c + 1) * 128], start=(c == 0), stop=(c == 3)),
                             r=["ones_b", "sq2"], w=[pk(1)])
                    P.ACT(lambda e: e.activation(out=rv[:, 0, :], in_=PS[1][:, 0:128], func=AF.Sqrt, bias=eps_t[:], scale=1.0 / 512),
                          r=[pk(1), "eps_t"], w=["rv"])
                    P.DVE(lambda e: e.reciprocal(out=rv[:, 1, :], in_=rv[:, 0, :]), r=["rv"], w=["rv"])
                    for c in range(4):
                        P.DVE(lambda e, c=c: e.scalar_tensor_tensor(out=qlatn[:, c, :], in0=qlf[:, c * 128:(c + 1) * 128], scalar=qnw[:, c:c + 1],
                                                                   in1=rv[:, 1, :], op0=ALU.mult, op1=ALU.mult), r=["qlf", "rv", "vT"], w=["qlatn"])
                    for g2 in range(2):
                        for hl in range(4):
                            hd = g2 * 4 + hl
                            for c in range(4):
                                P.PE(lambda e, c=c, hd=hd, hl=hl, g2=g2: e.matmul(PS[2 + g2][:, hl * 128:(hl + 1) * 128], wqb[:, c, hd * 192:hd * 192 + 128],
                                                                                 qlatn[:, c, :], start=(c == 0), stop=(c == 3)),
                                     r=["wqb", "qlatn"], w=[pk(2 + g2)])
                        P.ACT(lambda e, g2=g2, tsl=tsl: e.activation(out=qnT[:, g2 * 4:(g2 + 1) * 4, tsl],
                                                                    in_=PS[2 + g2][:, :].rearrange("p (c t) -> p c t", c=4), func=AF.Copy, scale=SC),
                              r=[pk(2 + g2)], w=[("qnT", tt)])
                    for v_ in range(2):
                        for g2 in range(2):
                            bank = (0 if v_ == 0 else 4) + g2
                            for hl in range(4):
                                hd = g2 * 4 + hl
                                base = hd * 192 + 128
                                for half in range(2):
                                    par = half if v_ == 0 else 1 - half
                                    for c in range(4):
                                        P.PE(lambda e, c=c, bank=bank, half=half, hl=hl, base=base, par=par: e.matmul(
                                            PS[bank][half * 32:(half + 1) * 32, hl * 128:(hl + 1) * 128], wqb[:, c, base + par:base + 64:2],
                                            qlatn[:, c, :], start=(c == 0), stop=(c == 3)), r=["wqb", "qlatn"], w=[pk(bank)])
                    for g2 in range(2):
                        cosb = cs_own[:, 0, tsl].unsqueeze(1).to_broadcast([64, 4, 128])
                        sinb = cs_own[:, 1, tsl].unsqueeze(1).to_broadcast([64, 4, 128])
                        v3 = lambda ap: ap.rearrange("p (c t) -> p c t", c=4)
                        P.DVE(lambda e, g2=g2, cosb=cosb: e.tensor_tensor(out=v3(rt[:, 0, :]), in0=v3(PS[g2][0:64, :]), in1=cosb, op=ALU.mult),
                              r=[pk(g2), "cs_own"], w=["rt0"])
                        P.DVE(lambda e, g2=g2, sinb=sinb: e.tensor_tensor(out=v3(rt[:, 1, :]), in0=v3(PS[4 + g2][0:64, :]), in1=sinb, op=ALU.mult),
                              r=[pk(4 + g2), "cs_own"], w=["rt1"])
                        P.DVE(lambda e: e.tensor_tensor(out=rt[:, 0, :], in0=rt[:, 0, :], in1=rt[:, 1, :], op=ALU.add), r=["rt0", "rt1"], w=["rt0"])
                        P.ACT(lambda e, g2=g2, tsl=tsl: e.activation(out=qrotT[0:64, g2 * 4:(g2 + 1) * 4, tsl], in_=v3(rt[:, 0, :]), func=AF.Copy, scale=SC),
                              r=["rt0"], w=[("qrotT", tt)])
                dump("d_glao", glaoT[:].rearrange("p a b -> p (a b)"), ("glaoT", 7), 8192)
                dump("d_qn", qnT[:].rearrange("p a b -> p (a b)"), ("qnT", 7), 8192)
                dump("d_qrot", qrotT[0:64, :, :].rearrange("p a b -> p (a b)"), ("qrotT", 7), 8192)
                P.barrier()

            if STOP <= 2:
                mixB.close()
                mix.close()
                return
            mixC = ExitStack()
            mlaoT = sb("mlaoT", [128, 8, 1024], BF16, mixC)
            with ExitStack() as ph:
                wkvb = sb("wkvb", [128, 2, 2048], BF16, ph)
                knT = sb("knT", [128, S], BF16, ph)
                Vh = sb("Vh", [128, NT, 128], BF16, ph)
                pT = [sb("pT%d" % i, [128, 512], BF16, ph) for i in range(2)]
                rs = sb("rs", [128, 512], F32, ph)
                P.dma("pool", wkvb[:], w_kv_b[:, :].rearrange("(k p) n -> p k n", p=128), w=["wkvb"])
                for h in range(8):
                    for sblk in range(8):
                        bank = sblk % 2
                        for c in range(2):
                            P.PE(lambda e, c=c, bank=bank, sblk=sblk, h=h: e.matmul(PS[bank][:, :], wkvb[:, c, h * 256:h * 256 + 128],
                                                                                  ckvn[:, c, sblk * 512:(sblk + 1) * 512], start=(c == 0), stop=(c == 1)),
                                 r=["wkvb"] + [("ckvn", t) for t in range(sblk * 4, sblk * 4 + 4)], w=[pk(bank)])
                        if sblk % 2 == 0:
                            P.ACT(lambda e, bank=bank, sblk=sblk: e.activation(out=knT[:, sblk * 512:(sblk + 1) * 512], in_=PS[bank][:, :], func=AF.Copy),
                                  r=[pk(bank)], w=[("knT", sblk)])
                        else:
                            P.DVE(lambda e, bank=bank, sblk=sblk: e.tensor_copy(out=knT[:, sblk * 512:(sblk + 1) * 512], in_=PS[bank][:, :]),
                                  r=[pk(bank)], w=[("knT", sblk)])
                    for g in range(8):
                        bank = 2 + g % 2
                        for i in range(4):
                            t = g * 4 + i
                            for c in range(2):
                                P.PE(lambda e, c=c, bank=bank, i=i, t=t, h=h: e.matmul(PS[bank][:, i * 128:(i + 1) * 128], ckvn[:, c, t * 128:(t + 1) * 128],
                                                                                     wkvb[:, c, h * 256 + 128:h * 256 + 256], start=(c == 0), stop=(c == 1)),
                                     r=["wkvb", ("ckvn", t)], w=[pk(bank)])
                        if g % 2 == 0:
                            P.ACT(lambda e, bank=bank, g=g: e.activation(out=Vh[:, g * 4:(g + 1) * 4, :], in_=PS[bank][:, :].rearrange("p (c t) -> p c t", c=4),
                                                                        func=AF.Copy), r=[pk(bank)], w=[("Vh", g)])
                        else:
                            P.DVE(lambda e, bank=bank, g=g: e.tensor_copy(out=Vh[:, g * 4:(g + 1) * 4, :], in_=PS[bank][:, :].rearrange("p (c t) -> p c t", c=4)),
                                  r=[pk(bank)], w=[("Vh", g)])
                    for qb in range(2):
                        nk = 28 + 4 * qb
                        qs = slice(qb * 512, (qb + 1) * 512)
                        qkeys = [("qnT", t) for t in range(qb * 4, qb * 4 + 4)]
                        qrkeys = [("qrotT", t) for t in range(qb * 4, qb * 4 + 4)]
                        def scores(kt):
                            sbk = kt % 2
                            ks = slice(kt * 128, (kt + 1) * 128)
                            P.PE(lambda e, sbk=sbk, ks=ks, h=h, qs=qs: e.matmul(PS[sbk][:, :], knT[:, ks], qnT[:, h, qs], start=True, stop=False),
                                 r=[("knT", kt // 4)] + qkeys, w=[pk(sbk)])
                            P.PE(lambda e, sbk=sbk, ks=ks, h=h, qs=qs: e.matmul(PS[sbk][:, :], krot[:, ks], qrotT[:, h, qs], start=False, stop=True),
                                 r=[("krot", kt), "krotpad", "qrotpad"] + qrkeys, w=[pk(sbk)])

                        scores(0)
                        for kt in range(nk):
                            sbk = kt % 2
                            if kt + 1 < nk:
                                scores(kt + 1)
                            P.ACT(lambda e, sbk=sbk, kt=kt: e.activation(out=pT[sbk][:], in_=PS[sbk][:, :], func=AF.Exp, bias=kbias[:, kt:kt + 1], scale=1.0),
                                  r=[pk(sbk), "kbias"], w=[("pT", sbk)])
                            m = kt - (24 + 4 * qb)
                            if m >= 0:
                                P.DVE(lambda e, sbk=sbk, m=m: e.tensor_tensor(out=pT[sbk][:], in0=pT[sbk][:], in1=cmask[:, m * 512:(m + 1) * 512], op=ALU.mult),
                                      r=[("pT", sbk), "cmask"], w=[("pT", sbk)])
                            P.PE(lambda e, sbk=sbk, kt=kt, qb=qb, nk=nk: e.matmul(PS[4 + qb][:, :], Vh[:, kt, :], pT[sbk][:], start=(kt == 0), stop=(kt == nk - 1)),
                                 r=[("Vh", kt // 4), ("pT", sbk)], w=[pk(4 + qb)])
                            P.PE(lambda e, sbk=sbk, kt=kt, qb=qb, nk=nk: e.matmul(PS[6 + qb][:, :], ones_b[:], pT[sbk][:], start=(kt == 0), stop=(kt == nk - 1)),
                                 r=["ones_b", ("pT", sbk)], w=[pk(6 + qb)])
                        P.DVE(lambda e, qb=qb: e.reciprocal(out=rs[:], in_=PS[6 + qb][:, :]), r=[pk(6 + qb)], w=["rs"])
                        P.DVE(lambda e, qb=qb, h=h, qs=qs: e.tensor_tensor(out=mlaoT[:, h, qs], in0=PS[4 + qb][:, :], in1=rs[:], op=ALU.mult),
                              r=[pk(4 + qb), "rs"], w=[("mlaoT", h)])
                dump("d_mlao", mlaoT[:].rearrange("p a b -> p (a b)"), ("mlaoT", 7), 8192)
                P.dma("sp", glao_d[:, :], glaoT[:].rearrange("p a b -> p (a b)"), r=[("glaoT", t) for t in range(8)], w=["glao_d"])
                P.dma("sp", mlao_d[:, :], mlaoT[:].rearrange("p a b -> p (a b)"), r=[("mlaoT", t) for t in range(8)], w=["mlao_d"])
                P.barrier()
            P.barrier()
            mixC.close()
            mixB.close()
            mix.close()

            if STOP <= 3:
                return

            def bc_row(dst, dkey, srcT, dg):
                for k in range(KD):
                    P.DVE(lambda e, k=k: e.tensor_scalar(out=dg[:], in0=ident[:], scalar1=srcT[:, k:k + 1], scalar2=None, op0=ALU.mult),
                          r=["ident", "modT", "AB"], w=["dg"])
                    bank = (k // 4) % 2
                    P.PE(lambda e, k=k, bank=bank: e.matmul(PS[bank][:, (k % 4) * 128:(k % 4 + 1) * 128], ones_f[:], dg[:], start=True, stop=True),
                         r=["ones_f", "dg"], w=[pk(bank)])
                    if k % 4 == 3:
                        P.ACT(lambda e, k=k, bank=bank: e.activation(out=dst[:, (k // 4) * 512:(k // 4 + 1) * 512], in_=PS[bank][:, :], func=AF.Copy),
                              r=[pk(bank)], w=[dkey])

            ffn = ExitStack()
            h2T = sb("h2T", [128, KD, 1024], BF16, ffn)
            with ExitStack() as ph4:
                mergedT = sb("mergedT", [128, KD, 1024], BF16, ph4)
                with ExitStack() as ph:
                    glaoT2 = sb("glaoT2", [128, 8, 1024], BF16, ph)
                    mlaoT2 = sb("mlaoT2", [128, 8, 1024], BF16, ph)
                    hTo = sb("hTo", [128, KD, 1024], BF16, ph)
                    xt = sb("xt4", [128, D], F32, ph)
                    junk = sb("junk4", [128, D], BF16, ph)
                    statt = sb("statt4", [128, 4], F32, ph)
                    stat = {"junk": junk, "t": statt}
                    wga = sb("wga", [128, KD, 512], BF16, ph)
                    wgb = sb("wgb", [128, KD, 512], BF16, ph)
                    woa = sb("woa", [128, 8, 512], BF16, ph)
                    wob = sb("wob", [128, 8, 512], BF16, ph)
                    sa_ = sb("sa_", [128, 512], F32, ph)
                    sb_ = sb("sb_", [128, 512], F32, ph)
                    P.dma("sp", glaoT2[:].rearrange("p a b -> p (a b)"), glao_d[:, :], r=["glao_d"], w=["glaoT2"])
                    P.dma("sp", mlaoT2[:].rearrange("p a b -> p (a b)"), mlao_d[:, :], r=["mlao_d"], w=["mlaoT2"])
                    for tt in range(8):
                        P.dma("sp", xt[:], xs[(OWN0 + tt) * 128:(OWN0 + tt + 1) * 128, :], w=["xt4"])
                        make_hT(xt[:], "xt4", lambda c, tt=tt: hTo[:, c, tt * 128:(tt + 1) * 128], [("hTo", tt, c) for c in range(KD)], A_m, B_m, stat)
                    for dcg in range(4):
                        cs_ = slice(dcg * 512, (dcg + 1) * 512)
                        P.dma("pool", wga[:], w_in[:, C_GA + dcg * 512:C_GA + (dcg + 1) * 512].rearrange("(k p) n -> p k n", p=128), w=["wga"])
                        P.dma("pool", wgb[:], w_in[:, C_GB + dcg * 512:C_GB + (dcg + 1) * 512].rearrange("(k p) n -> p k n", p=128), w=["wgb"])
                        P.dma("pool", woa[:], w_o_mla[:, cs_].rearrange("(k p) n -> p k n", p=128), w=["woa"])
                        P.dma("pool", wob[:], w_o_gla[:, cs_].rearrange("(k p) n -> p k n", p=128), w=["wob"])
                        for dcl in range(4):
                            dc = dcg * 4 + dcl
                            ws = slice(dcl * 128, (dcl + 1) * 128)
                            for th in range(2):
                                ts_ = slice(th * 512, (th + 1) * 512)
                                pb = 4 * ((dc * 2 + th) % 2)
                                for k in range(KD):
                                    P.PE(lambda e, k=k, ws=ws, ts_=ts_, pb=pb: e.matmul(PS[pb + 0][:, :], wga[:, k, ws], hTo[:, k, ts_], start=(k == 0), stop=(k == KD - 1)),
                                         r=["wga"] + [("hTo", t, k) for t in range(th * 4, th * 4 + 4)], w=[pk(pb + 0)])
                                for k in range(KD):
                                    P.PE(lambda e, k=k, ws=ws, ts_=ts_, pb=pb: e.matmul(PS[pb + 1][:, :], wgb[:, k, ws], hTo[:, k, ts_], start=(k == 0), stop=(k == KD - 1)),
                                         r=["wgb"] + [("hTo", t, k) for t in range(th * 4, th * 4 + 4)], w=[pk(pb + 1)])
                                for c in range(8):
                                    P.PE(lambda e, c=c, ws=ws, ts_=ts_, pb=pb: e.matmul(PS[pb + 2][:, :], woa[:, c, ws], mlaoT2[:, c, ts_], start=(c == 0), stop=(c == 7)),
                                         r=["woa", "mlaoT2"], w=[pk(pb + 2)])
                                for c in range(8):
                                    P.PE(lambda e, c=c, ws=ws, ts_=ts_, pb=pb: e.matmul(PS[pb + 3][:, :], wob[:, c, ws], glaoT2[:, c, ts_], start=(c == 0), stop=(c == 7)),
                                         r=["wob", "glaoT2"], w=[pk(pb + 3)])
                                P.ACT(lambda e, pb=pb: e.activation(out=sa_[:], in_=PS[pb + 0][:, :], func=AF.Sigmoid), r=[pk(pb + 0)], w=["sa_"])
                                P.ACT(lambda e, pb=pb: e.activation(out=sb_[:], in_=PS[pb + 1][:, :], func=AF.Sigmoid), r=[pk(pb + 1)], w=["sb_"])
                                P.DVE(lambda e, pb=pb: e.tensor_tensor(out=sa_[:], in0=sa_[:], in1=PS[pb + 2][:, :], op=ALU.mult), r=["sa_", pk(pb + 2)], w=["sa_"])
                                P.DVE(lambda e, pb=pb: e.tensor_tensor(out=sb_[:], in0=sb_[:], in1=PS[pb + 3][:, :], op=ALU.mult), r=["sb_", pk(pb + 3)], w=["sb_"])
                                P.DVE(lambda e, dc=dc, ts_=ts_: e.tensor_tensor(out=mergedT[:, dc, ts_], in0=sa_[:], in1=sb_[:], op=ALU.add),
                                      r=["sa_", "sb_"], w=[("mergedT", dc, th)])
                    dump("d_merged", mergedT[:].rearrange("p a b -> p (a b)"), ("mergedT", 15, 1), 16384)
                    P.barrier()
                with ExitStack() as ph:
                    wout = sb("wout", [128, KD, D], BF16, ph)
                    wr = sb("wr", [128, KD, NE], F32, ph)
                    gmbc = sb("gmbc", [128, D], F32, ph)
                    dg = sb("dg", [128, 128], F32, ph)
                    xt = sb("xt4b", [128, D], F32, ph)
                    x1t = sb("x1t", [128, D], F32, ph)
                    h2f = sb("h2f", [128, KD, 128], F32, ph)
                    junk = sb("junk4b", [128, D], BF16, ph)
                    statt = sb("statt4b", [128, 4], F32, ph)
                    stat = {"junk": junk, "t": statt}
                    gt = sb("gt", [128, 4, NE], F32, ph)
                    gs8 = sb("gs8", [128, 16], F32, ph)
                    for q4 in range(4):
                        P.dma("pool", wout[:, :, q4 * 512:(q4 + 1) * 512], w_out[:, q4 * 512:(q4 + 1) * 512].rearrange("(k p) n -> p k n", p=128),
                              w=["wout"], semkey="wout")
                    P.finalize("wout")
                    P.dma("sp", wr[:], w_router[:, :].rearrange("(k p) n -> p k n", p=128), w=["wr"])
                    bc_row(gmbc, "gmbc", modT[:, 32:48], dg)
                    for tt in range(8):
                        tsl = slice(tt * 128, (tt + 1) * 128)
                        P.dma("sp", xt[:], xs[(OWN0 + tt) * 128:(OWN0 + tt + 1) * 128, :], w=["xt4b"])
                        for dt_ in range(4):
                            bank = dt_ % 2
                            ds_ = slice(dt_ * 512, (dt_ + 1) * 512)
                            for k in range(KD):
                                P.PE(lambda e, k=k, bank=bank, tsl=tsl, ds_=ds_: e.matmul(PS[bank][:, :], mergedT[:, k, tsl], wout[:, k, ds_],
                                                                                         start=(k == 0), stop=(k == KD - 1)),
                                     r=["wout", ("mergedT", k, tt // 4)], w=[pk(bank)])
                            P.DVE(lambda e, bank=bank, ds_=ds_: e.tensor_tensor(out=x1t[:, ds_], in0=PS[bank][:, :], in1=gmbc[:, ds_], op=ALU.mult),
                                  r=[pk(bank), "gmbc"], w=["x1t"])
                            P.DVE(lambda e, ds_=ds_: e.tensor_tensor(out=x1t[:, ds_], in0=x1t[:, ds_], in1=xt[:, ds_], op=ALU.add),
                                  r=["x1t", "xt4b"], w=["x1t"])
                        P.dma("sp", x1d[tsl, :], x1t[:], r=["x1t"], w=[("x1d", tt)], semkey="x1st")
                        if DEBUG:
                            P.dma("pool", dbg["d_x1"][:, tt * 2048:(tt + 1) * 2048], x1t[:], r=["x1t"], semkey=("dbg", "d_x1"))
                        make_hT(x1t[:], "x1t", lambda c, tsl=tsl: h2T[:, c, tsl], [("h2T", tt, c) for c in range(KD)], A_f, B_f, stat,
                                f32dst_fn=lambda c: h2f[:, c, :], f32keys=[("h2f", c) for c in range(KD)])
                        for k in range(KD):
                            P.PE(lambda e, k=k: e.matmul(PS[2][:, 0:NE], h2f[:, k, :], wr[:, k, :], start=(k == 0), stop=(k == KD - 1)),
                                 r=[("h2f", k), "wr"], w=[pk(2)])
                        lg, msk, ex = gt[:, 0, :], gt[:, 1, :], gt[:, 2, :]
                        P.DVE(lambda e: e.tensor_tensor(out=lg, in0=PS[2][:, 0:NE], in1=brout[:], op=ALU.add), r=[pk(2), "brout"], w=["lg"])
                        P.DVE(lambda e: e.max(out=gs8[:, 0:8], in_=lg), r=["lg"], w=["mx8"])
                        P.DVE(lambda e: e.tensor_scalar(out=msk, in0=lg, scalar1=gs8[:, 3:4], scalar2=None, op0=ALU.is_ge), r=["lg", "mx8"], w=["msk"])
                        P.DVE(lambda e: e.tensor_scalar(out=gs8[:, 8:9], in0=gs8[:, 0:1], scalar1=-1.0, scalar2=None, op0=ALU.mult), r=["mx8"], w=["nmx"])
                        P.ACT(lambda e: e.activation(out=ex, in_=lg, func=AF.Exp, bias=gs8[:, 8:9], scale=1.0), r=["lg", "nmx"], w=["ex"])
                        P.DVE(lambda e: e.tensor_tensor(out=ex, in0=ex, in1=msk, op=ALU.mult), r=["ex", "msk"], w=["ex"])
                        P.DVE(lambda e: e.tensor_reduce(out=gs8[:, 9:10], in_=ex, axis=AX.X, op=ALU.add), r=["ex"], w=["ssum"])
                        P.DVE(lambda e: e.reciprocal(out=gs8[:, 10:11], in_=gs8[:, 9:10]), r=["ssum"], w=["rsum"])
                        P.DVE(lambda e, tt=tt: e.tensor_scalar(out=G[:, tt, :], in0=ex, scalar1=gs8[:, 10:11], scalar2=None, op0=ALU.mult),
                              r=["ex", "rsum"], w=[("G", tt)])
                    P.finalize("x1st")
                    dump("d_G", G[:].rearrange("p a b -> p (a b)"), ("G", 7), 256)
                    P.barrier()
            P.barrier()

            if STOP <= 4:
                ffn.close()
                return
            moe = ExitStack()
            acc = sb("acc", [128, 8, D], F32, moe)
            b1T = sb("b1T", [128, 32, NE], F32, moe)
            gTb = sb("gTb", [32, 8, 128], BF16, moe)
            b2b = sb("b2b", [32, D], BF16, moe)
            with ExitStack() as ph:
                b1s = sb("b1s", [32, 2 * D], F32, ph)
                P.dma("sp", b1s[:], b1[:, :], w=["b1s"])
                for c2 in range(32):
                    c, par = c2 // 2, c2 % 2
                    bank = c2 // 16
                    P.PE(lambda e, c=c, par=par, bank=bank, c2=c2: e.transpose(PS[bank][:, (c2 % 16) * 32:(c2 % 16 + 1) * 32],
                                                                             b1s[:, 256 * c + par:256 * c + 256:2], ident[0:32, 0:32]),
                         r=["b1s", "ident"], w=[pk(bank)])
                for bank in range(2):
                    P.ACT(lambda e, bank=bank: e.activation(out=b1T[:, bank * 16:(bank + 1) * 16, :], in_=PS[bank][:, :].rearrange("p (c e) -> p c e", c=16),
                                                           func=AF.Copy), r=[pk(bank)], w=["b1T"])
                P.barrier()
            P.dma("pool", b2b[:], b2[:, :], w=["b2b"])
            for tt in range(8):
                bank = 2 + tt // 4
                P.PE(lambda e, tt=tt, bank=bank: e.transpose(PS[bank][0:32, (tt % 4) * 128:(tt % 4 + 1) * 128], G[:, tt, :], ident[:]),
                     r=[("G", tt), "ident"], w=[pk(bank)])
            for bank in range(2):
                P.ACT(lambda e, bank=bank: e.activation(out=gTb[:, bank * 4:(bank + 1) * 4, :], in_=PS[2 + bank][0:32, :].rearrange("p (c t) -> p c t", c=4),
                                                       func=AF.Copy), r=[pk(2 + bank)], w=["gTb"])
            for tt in range(8):
                for dt_ in range(4):
                    bank = 4 + dt_ % 2
                    ds_ = slice(dt_ * 512, (dt_ + 1) * 512)
                    P.PE(lambda e, tt=tt, bank=bank, ds_=ds_: e.matmul(PS[bank][:, :], gTb[:, tt, :], b2b[:, ds_], start=True, stop=True),
                         r=["gTb", "b2b"], w=[pk(bank)])
                    P.ACT(lambda e, tt=tt, bank=bank, ds_=ds_: e.activation(out=acc[:, tt, ds_], in_=PS[bank][:, :], func=AF.Copy),
                          r=[pk(bank)], w=[("acc", tt, dt_)])
            with ExitStack() as ph:
                actT = sb("actT", [128, KD, 1024], BF16, ph)
                w1b = [sb("w1b%d" % i, [128, KD, 256], BF16, ph) for i in range(2)]
                w2b = [sb("w2b%d" % i, [128, KD, 512], BF16, ph) for i in range(2)]
                tg = sb("tg", [128, 512], F32, ph)
                tsg = sb("tsg", [128, 512], F32, ph)
                tl = sb("tl", [128, 512], F32, ph)
                NX = N_EXPERTS_RUN
                L1 = [(e_, c) for e_ in range(NX) for c in range(16)]
                L2 = [(e_, d_) for e_ in range(NX) for d_ in range(4)]

                def load1(i):
                    e_, c = L1[i]
                    P.dma("pool", w1b[i % 2][:], w1[e_, :, 256 * c:256 * (c + 1)].rearrange("(k p) n -> p k n", p=128), w=[("w1b", i % 2)])

                def load2(i):
                    e_, d_ = L2[i]
                    P.dma("pool", w2b[i % 2][:], w2[e_, :, 512 * d_:512 * (d_ + 1)].rearrange("(k p) n -> p k n", p=128), w=[("w2b", i % 2)])

                load1(0)
                load2(0)
                for e_ in range(NX):
                    for c in range(16):
                        i1 = e_ * 16 + c
                        if i1 + 1 < len(L1):
                            load1(i1 + 1)
                        wb = w1b[i1 % 2]
                        wkey = ("w1b", i1 % 2)
                        for th in range(2):
                            ts_ = slice(th * 512, (th + 1) * 512)
                            pg, pl = 2 * th, 2 * th + 1
                            for par, bank in ((0, pg), (1, pl)):
                                for k in range(KD):
                                    P.PE(lambda e, k=k, par=par, bank=bank, wb=wb, ts_=ts_: e.matmul(PS[bank][:, :], wb[:, k, par:256:2], h2T[:, k, ts_],
                                                                                                start=(k == 0), stop=(k == KD - 1)),
                                         r=[wkey] + [("h2T", t, k) for t in range(th * 4, th * 4 + 4)], w=[pk(bank)])
                            P.DVE(lambda e, pg=pg, c=c, e_=e_: e.tensor_scalar(out=tg[:], in0=PS[pg][:, :], scalar1=b1T[:, 2 * c, e_:e_ + 1], scalar2=7.0,
                                                                             op0=ALU.add, op1=ALU.min), r=[pk(pg), "b1T"], w=["tg"])
                            P.ACT(lambda e: e.activation(out=tsg[:], in_=tg[:], func=AF.Sigmoid, scale=1.702), r=["tg"], w=["tsg"])
                            P.DVE(lambda e, pl=pl, c=c, e_=e_: e.tensor_scalar(out=tl[:], in0=PS[pl][:, :], scalar1=b1T[:, 2 * c + 1, e_:e_ + 1], scalar2=7.0,
                                                                             op0=ALU.add, op1=ALU.min), r=[pk(pl), "b1T"], w=["tl"])
                            P.DVE(lambda e: e.tensor_scalar(out=tl[:], in0=tl[:], scalar1=-7.0, scalar2=1.0, op0=ALU.max, op1=ALU.add), r=["tl"], w=["tl"])
                            P.DVE(lambda e: e.tensor_tensor(out=tg[:], in0=tg[:], in1=tsg[:], op=ALU.mult), r=["tg", "tsg"], w=["tg"])
                            P.DVE(lambda e, c=c, ts_=ts_: e.tensor_tensor(out=actT[:, c, ts_], in0=tg[:], in1=tl[:], op=ALU.mult),
                                  r=["tg", "tl"], w=[("actT", c, th)])
                    for d_ in range(4):
                        i2 = e_ * 4 + d_
                        if i2 + 1 < len(L2):
                            load2(i2 + 1)
                        wb = w2b[i2 % 2]
                        wkey = ("w2b", i2 % 2)
                        ds_ = slice(d_ * 512, (d_ + 1) * 512)
                        for tt in range(8):
                            bank = 4 + tt % 2
                            tsl = slice(tt * 128, (tt + 1) * 128)
                            for c in range(KD):
                                P.PE(lambda e, c=c, bank=bank, wb=wb, tsl=tsl: e.matmul(PS[bank][:, :], actT[:, c, tsl], wb[:, c, :],
                                                                                      start=(c == 0), stop=(c == KD - 1)),
                                     r=[wkey, ("actT", c, tt // 4)], w=[pk(bank)])
                            P.DVE(lambda e, bank=bank, tt=tt, ds_=ds_, e_=e_: e.scalar_tensor_tensor(out=acc[:, tt, ds_], in0=PS[bank][:, :],
                                                                                                  scalar=G[:, tt, e_:e_ + 1], in1=acc[:, tt, ds_],
                                                                                                  op0=ALU.mult, op1=ALU.add),
                                  r=[pk(bank), ("G", tt), ("acc", tt, d_)], w=[("acc", tt, d_)])
                P.barrier()
            if DEBUG:
                for tt in range(8):
                    P.dma("pool", dbg["d_acc"][:, tt * 2048:(tt + 1) * 2048], acc[:, tt, :], r=[("acc", tt, d_) for d_ in range(4)], semkey=("dbg", "d_acc"))

            with ExitStack() as ph:
                gfbc = sb("gfbc", [128, D], F32, ph)
                aobc = sb("aobc", [128, D], F32, ph)
                bobc = sb("bobc", [128, D], F32, ph)
                dg = sb("dg7", [128, 128], F32, ph)
                x1t = sb("x1t7", [128, D], F32, ph)
                yt = [sb("yt%d" % i, [128, D], F32, ph) for i in range(2)]
                junk = sb("junk7", [128, D], BF16, ph)
                st7 = sb("st7", [128, 4], F32, ph)
                bc_row(gfbc, "gfbc", modT[:, 80:96], dg)
                bc_row(aobc, "aobc", A_o, dg)
                bc_row(bobc, "bobc", B_o, dg)
                for tt in range(8):
                    tsl = slice(tt * 128, (tt + 1) * 128)
                    y = yt[tt % 2]
                    yk = ("yt", tt % 2)
                    P.dma("sp", x1t[:], x1d[tsl, :], r=[("x1d", tt)], w=["x1t7"])
                    P.DVE(lambda e, tt=tt, y=y: e.tensor_tensor(out=y[:], in0=acc[:, tt, :], in1=gfbc[:], op=ALU.mult),
                          r=[("acc", tt, d_) for d_ in range(4)] + ["gfbc"], w=[yk])
                    P.DVE(lambda e, y=y: e.tensor_tensor(out=y[:], in0=y[:], in1=x1t[:], op=ALU.add), r=[yk, "x1t7"], w=[yk])
                    P.ACT(lambda e, y=y: e.activation(out=junk[:], in_=y[:], func=AF.Square, accum_out=st7[:, 0:1]), r=[yk], w=["junk7", "st7"])
                    P.ACT(lambda e: e.activation(out=st7[:, 1:2], in_=st7[:, 0:1], func=AF.Sqrt, bias=eps_t[:], scale=1.0 / D), r=["st7", "eps_t"], w=["st7"])
                    P.DVE(lambda e: e.reciprocal(out=st7[:, 2:3], in_=st7[:, 1:2]), r=["st7"], w=["st7"])
                    P.DVE(lambda e, y=y: e.scalar_tensor_tensor(out=y[:], in0=y[:], scalar=st7[:, 2:3], in1=aobc[:], op0=ALU.mult, op1=ALU.mult),
                          r=[yk, "st7", "aobc"], w=[yk])
                    P.DVE(lambda e, y=y: e.tensor_tensor(out=y[:], in0=y[:], in1=bobc[:], op=ALU.add), r=[yk, "bobc"], w=[yk])
                    P.dma("sp", out[tsl, :], y[:], r=[yk], w=[("out", tt)], semkey="outst")
                P.barrier()
            moe.close()
            ffn.close()

        block.sync(body)
        P.final_wait()
    return nc


def _consts():
    ident = np.eye(128, dtype=np.float32)
    tp = np.arange(128)[:, None]
    tt = np.arange(128)[None, :]
    tri = np.where(tp <= tt, -1.0 / 16, 0.0).astype(np.float32)
    suf = np.where(tp > tt, -1.0 / 16, 0.0).astype(np.float32)
    c_tri = np.concatenate([tri, suf], axis=1)
    f = np.arange(512)[None, :]
    p = np.arange(128)[:, None]
    c_mask = np.concatenate([(f - p - 128 * m >= 0).astype(np.float32) for m in range(4)], axis=1)
    inv = (10000.0 ** (-(np.arange(0, 64, 2, dtype=np.float32) / 64))).astype(np.float32)
    c_rope = np.zeros((64, 2), np.float32)
    c_rope[:, 0] = np.concatenate([inv, inv])
    c_rope[:, 1] = np.concatenate([-np.ones(32), np.ones(32)])
    return dict(c_ident=ident, c_tri=np.ascontiguousarray(c_tri), c_mask=np.ascontiguousarray(c_mask), c_rope=c_rope)


def _prep_inputs(inp):
    f = lambda a: np.ascontiguousarray(np.asarray(a))
    x = f(inp["x"]); c = f(inp["c"]); pos = f(inp["positions"])
    shared = dict(
        w_ada=f(inp["w_ada"][0]), b_ada=f(inp["b_ada"][0]).reshape(96, 128),
        w_ada_final=f(inp["w_ada_final"]), b_ada_final=f(inp["b_ada_final"]).reshape(32, 128),
        vecs=np.concatenate([f(inp["norm_mix_w"][0]).reshape(16, 128), f(inp["norm_ffn_w"][0]).reshape(16, 128),
                             f(inp["norm_final_w"]).reshape(16, 128), f(inp["mla_q_norm_w"][0]).reshape(4, 128),
                             f(inp["mla_kv_norm_w"][0]).reshape(2, 128)], axis=0),
        w_in=f(inp["w_in"][0]), mla_w_q_b=f(inp["mla_w_q_b"][0]), mla_w_kv_b=f(inp["mla_w_kv_b"][0]),
        wgk1=np.concatenate([f(inp["gla_w_gk_up"][0]), f(inp["gla_b_gk_up"][0]).reshape(1, 512)], axis=0),
        gla_nw=np.ascontiguousarray(np.broadcast_to(f(inp["gla_norm_w"][0]).reshape(1, 256), (128, 256))),
        w_o_mla=f(inp["w_o_mla"][0]), w_o_gla=f(inp["w_o_gla"][0]), w_out=f(inp["w_out"][0]),
        w_router=f(inp["w_router"][0]),
        b_router=np.ascontiguousarray(np.broadcast_to(f(inp["b_router"][0]).reshape(1, NE), (128, NE))),
        w1=f(inp["w1"][0]), b1=f(inp["b1"][0]), w2=f(inp["w2"][0]), b2=f(inp["b2"][0]),
    )
    shared.update(_consts())
    in_maps = []
    for core in range(8):
        b, j = core // 4, core % 4
        npad = (3 - j) * 1024
        xs = np.zeros((S, D), np.float32)
        xs[npad:] = x[b, : (j + 1) * 1024]
        ps = np.zeros((S,), np.int32)
        ps[npad:] = pos[b, : (j + 1) * 1024]
        slot = np.arange(S).reshape(NT, 128).T
        m = dict(shared)
        m["xs"] = xs
        m["posr"] = np.ascontiguousarray(np.broadcast_to(ps[None, :], (64, S)))
        m["valid"] = (slot >= npad).astype(np.float32)
        m["cT"] = np.ascontiguousarray(c[b].reshape(KD, 128).T)
        in_maps.append(m)
    return in_maps


def kernel(**inputs):
    in_maps = _prep_inputs(inputs)
    if STOP <= 4:
        in_maps = [{k: v for k, v in m.items() if k not in ("w1", "b1", "w2", "b2")} for m in in_maps]
    nc = build_nc()
    res = run_bass_kernel_spmd(nc, in_maps, core_ids=list(range(8)))
    if DEBUG:
        kernel.last = res
    outs = [r["out"] for r in res.results]
    full = np.zeros((2, 4096, D), np.float32)
    for core in range(8):
        b, j = core // 4, core % 4
        full[b, j * 1024:(j + 1) * 1024] = outs[core]
    return full
```
